# Optimizing a Trainium2 kernel written in Bass

```python
import math
import jax, jax.numpy as jnp
from jax import lax
import numpy as np

D_MODEL = 1024
BATCH = 2
SEQ = 8192
DEPTH = 4

N_A = DEPTH // 2
N_B = DEPTH - N_A
CONV_WIDTH = 31
N_HEADS = 8
HEAD_DIM = 64
V_DIM = 2 * HEAD_DIM
QK_WIDTH = 2 * N_HEADS * HEAD_DIM
V_WIDTH = N_HEADS * V_DIM
N_BUCKETS = 32
MAX_DISTANCE = 128
Q_BLOCK = 128
N_EXPERTS = 16
N_GROUPS = 4
EXPERTS_PER_GROUP = N_EXPERTS // N_GROUPS
TOP_K = 2
D_EXPERT = 512
ALPHA = (2.0 * DEPTH) ** 0.25
BETA = (8.0 * DEPTH) ** -0.25
LN_EPS = 1e-5

kernel_name = "yoco_conformer_diffattn_grouped_moe"


def layer_norm(x, g, b):
    xf = x.astype(jnp.float32)
    mu = jnp.mean(xf, -1, keepdims=True)
    var = jnp.mean(jnp.square(xf - mu), -1, keepdims=True)
    return ((xf - mu) * lax.rsqrt(var + LN_EPS) * g.astype(jnp.float32) + b.astype(jnp.float32)).astype(x.dtype)


def conformer_conv(x, w_pw1, b_pw1, w_dw, b_dw, g, b, w_pw2, b_pw2):
    h = x @ w_pw1 + b_pw1
    a, gate = jnp.split(h, 2, axis=-1)
    h = a * jax.nn.sigmoid(gate)
    h = lax.conv_general_dilated(
        h, w_dw[:, None, :].astype(h.dtype), window_strides=(1,),
        padding=[(CONV_WIDTH - 1, 0)],
        dimension_numbers=("NWC", "WIO", "NWC"),
        feature_group_count=D_MODEL) + b_dw
    h = jax.nn.silu(layer_norm(h, g, b))
    return h @ w_pw2 + b_pw2


def t5_bucket(n):
    n = jnp.maximum(n, 0)
    max_exact = N_BUCKETS // 2
    nf = jnp.maximum(n, 1).astype(jnp.float32)
    large = max_exact + (jnp.log(nf / max_exact) / math.log(MAX_DISTANCE / max_exact)
                         * (N_BUCKETS - max_exact)).astype(jnp.int32)
    large = jnp.minimum(large, N_BUCKETS - 1)
    return jnp.where(n < max_exact, n, large)


def diff_attention(x, k1, k2, v, w_q, lam_params, subln_g, w_o, rel_bias, layer_idx):
    bsz, seq, _ = x.shape
    n_blk = seq // Q_BLOCK
    lambda_init = 0.8 - 0.6 * math.exp(-0.3 * layer_idx)
    lp = lam_params.astype(jnp.float32)
    lam = jnp.exp(jnp.sum(lp[0] * lp[1])) - jnp.exp(jnp.sum(lp[2] * lp[3])) + lambda_init
    q = (x @ w_q).reshape(bsz, n_blk, Q_BLOCK, 2, N_HEADS, HEAD_DIM)
    q = jnp.transpose(q, (3, 1, 0, 4, 2, 5))
    scale = HEAD_DIM ** -0.5
    k_pos = jnp.arange(seq)

    def block(args):
        qb1, qb2, i = args
        q_pos = i * Q_BLOCK + jnp.arange(Q_BLOCK)
        rel = q_pos[:, None] - k_pos[None, :]
        bias = jnp.transpose(jnp.take(rel_bias, t5_bucket(rel), axis=0), (2, 0, 1)).astype(jnp.float32)
        causal = rel >= 0

        def probs(qb, k):
            s = jnp.einsum("bhqd,bhkd->bhqk", qb, k).astype(jnp.float32) * scale + bias
            return jax.nn.softmax(jnp.where(causal, s, -jnp.inf), axis=-1)

        a = probs(qb1, k1) - lam * probs(qb2, k2)
        return jnp.einsum("bhqk,bhkv->bhqv", a.astype(v.dtype), v)

    out = lax.map(block, (q[0], q[1], jnp.arange(n_blk)))
    out = jnp.transpose(out, (1, 0, 3, 2, 4)).reshape(bsz, seq, N_HEADS, V_DIM)
    of = out.astype(jnp.float32)
    of = of * lax.rsqrt(jnp.mean(jnp.square(of), -1, keepdims=True) + LN_EPS) * subln_g.astype(jnp.float32)
    of = of * (1.0 - lambda_init)
    return of.astype(x.dtype).reshape(bsz, seq, V_WIDTH) @ w_o


def grouped_moe(x, router_w, router_bias, w_gate, w_up, w_down):
    bsz, seq, d = x.shape
    x2 = x.reshape(-1, d)
    aff = jax.nn.sigmoid((x2 @ router_w).astype(jnp.float32))
    sel = (aff + router_bias.astype(jnp.float32)).reshape(-1, N_GROUPS, EXPERTS_PER_GROUP)
    grp_score = jnp.sum(lax.top_k(sel, 2)[0], -1)
    g_idx = jnp.argmax(grp_score, -1)
    in_grp = jnp.take_along_axis(sel, g_idx[:, None, None], axis=1)[:, 0]
    _, local = lax.top_k(in_grp, TOP_K)
    e_idx = g_idx[:, None] * EXPERTS_PER_GROUP + local
    w = jnp.take_along_axis(aff, e_idx, -1)
    w = w / jnp.sum(w, -1, keepdims=True)
    gates = jnp.sum(jax.nn.one_hot(e_idx, N_EXPERTS, dtype=jnp.float32) * w[..., None], axis=1)

    def step(acc, ew):
        g_e, wg_e, wu_e, wd_e = ew
        h = jax.nn.silu(x2 @ wg_e) * (x2 @ wu_e)
        return acc + (h @ wd_e) * g_e[:, None].astype(x2.dtype), None

    out, _ = lax.scan(step, jnp.zeros_like(x2), (gates.T, w_gate, w_up, w_down))
    return out.reshape(bsz, seq, d)


def setup_inputs(seed: int = 0) -> dict:
    key = jax.random.key(seed)
    ks = jax.random.split(key, 32)
    nrm = jax.random.normal
    D, f32 = D_MODEL, jnp.float32
    w_kv_k = nrm(ks[9], (D, QK_WIDTH), f32) * D ** -0.5
    w_kv_v = nrm(ks[10], (D, V_WIDTH), f32) * D ** -0.5 * BETA
    return {
        "x": nrm(ks[0], (BATCH, SEQ, D), f32),
        "a_w_pw1": nrm(ks[1], (N_A, D, 2 * D), f32) * D ** -0.5,
        "a_b_pw1": 0.02 * nrm(ks[2], (N_A, 2 * D), f32),
        "a_w_dw": nrm(ks[3], (N_A, CONV_WIDTH, D), f32) * CONV_WIDTH ** -0.5,
        "a_b_dw": 0.02 * nrm(ks[4], (N_A, D), f32),
        "a_ln_g": 1.0 + 0.02 * nrm(ks[5], (N_A, D), f32),
        "a_ln_b": 0.02 * nrm(ks[6], (N_A, D), f32),
        "a_w_pw2": nrm(ks[7], (N_A, D, D), f32) * D ** -0.5 * BETA,
        "a_b_pw2": 0.02 * nrm(ks[8], (N_A, D), f32),
        "w_kv": jnp.concatenate([w_kv_k, w_kv_v], axis=1),
        "b_w_q": nrm(ks[11], (N_B, D, QK_WIDTH), f32) * D ** -0.5,
        "b_lambda": 0.1 * nrm(ks[12], (N_B, 4, HEAD_DIM), f32),
        "b_subln_g": 1.0 + 0.02 * nrm(ks[13], (N_B, V_DIM), f32),
        "b_w_o": nrm(ks[14], (N_B, V_WIDTH, D), f32) * V_WIDTH ** -0.5 * BETA,
        "rel_bias": 0.5 * nrm(ks[15], (N_BUCKETS, N_HEADS), f32),
        "ln_mix_g": 1.0 + 0.02 * nrm(ks[16], (DEPTH, D), f32),
        "ln_mix_b": 0.02 * nrm(ks[17], (DEPTH, D), f32),
        "ln_ffn_g": 1.0 + 0.02 * nrm(ks[18], (DEPTH, D), f32),
        "ln_ffn_b": 0.02 * nrm(ks[19], (DEPTH, D), f32),
        "router_w": nrm(ks[20], (D, N_EXPERTS), f32) * D ** -0.5,
        "router_bias": 0.01 * nrm(ks[21], (N_EXPERTS,), f32),
        "moe_w_gate": nrm(ks[22], (DEPTH, N_EXPERTS, D, D_EXPERT), f32) * D ** -0.5,
        "moe_w_up": nrm(ks[23], (DEPTH, N_EXPERTS, D, D_EXPERT), f32) * D ** -0.5,
        "moe_w_down": nrm(ks[24], (DEPTH, N_EXPERTS, D_EXPERT, D), f32) * D_EXPERT ** -0.5 * BETA,
    }


def reference(x, a_w_pw1, a_b_pw1, a_w_dw, a_b_dw, a_ln_g, a_ln_b, a_w_pw2, a_b_pw2,
              w_kv, b_w_q, b_lambda, b_subln_g, b_w_o, rel_bias,
              ln_mix_g, ln_mix_b, ln_ffn_g, ln_ffn_b,
              router_w, router_bias, moe_w_gate, moe_w_up, moe_w_down):
    bsz, seq, _ = x.shape
    k1 = k2 = v = None
    for l in range(DEPTH):
        if l < N_A:
            mix = conformer_conv(x, a_w_pw1[l], a_b_pw1[l], a_w_dw[l], a_b_dw[l],
                                 a_ln_g[l], a_ln_b[l], a_w_pw2[l], a_b_pw2[l])
        else:
            if l == N_A:
                kv = x @ w_kv
                k = kv[..., :QK_WIDTH].reshape(bsz, seq, 2, N_HEADS, HEAD_DIM)
                k = jnp.transpose(k, (2, 0, 3, 1, 4))
                k1, k2 = k[0], k[1]
                v = jnp.transpose(kv[..., QK_WIDTH:].reshape(bsz, seq, N_HEADS, V_DIM), (0, 2, 1, 3))
            j = l - N_A
            mix = diff_attention(x, k1, k2, v, b_w_q[j], b_lambda[j], b_subln_g[j], b_w_o[j], rel_bias, l)
        x = layer_norm(ALPHA * x + mix, ln_mix_g[l], ln_mix_b[l])
        ffn = grouped_moe(x, router_w, router_bias, moe_w_gate[l], moe_w_up[l], moe_w_down[l])
        x = layer_norm(ALPHA * x + ffn, ln_ffn_g[l], ln_ffn_b[l])
    return x
```

```python
import math
import os
from contextlib import ExitStack
CUT = int(os.environ.get("KCUT", "99"))
RCUT = int(os.environ.get("RCUT", "99"))

import numpy as np
import ml_dtypes

import concourse.bass as bass
import concourse.mybir as mybir
from concourse.bass_utils import run_bass_kernel_spmd

F32 = mybir.dt.float32
BF16 = mybir.dt.bfloat16
AF = mybir.ActivationFunctionType
ALU = mybir.AluOpType
AX = mybir.AxisListType

NCORES = 8
D = 1024
SEQ = 8192
BATCH = 2
DEPTH = 4
N_A = 2
KC = 8
NPIECE = 4
NT_MAIN = 16
NT_HALO = 4
CONVW = 31
NE = 16
DEXP = 512
NH = 8
HD = 64
VD = 128
ALPHA = (2.0 * DEPTH) ** 0.25
LN_EPS = 1e-5
NEG = -30000.0
KVROWS = 4096
KVSPLIT = 8


def piece_positions(r):
    return [r, 7 - r, 8 + r, 15 - r]


class Sched:
    LIMIT = 60000

    def __init__(self, nc, es, ndma=48, strict=("act", "dve", "pool")):
        self.nc = nc
        self.es = es
        self.engs = {"pe": nc.tensor, "act": nc.scalar, "dve": nc.vector,
                     "pool": nc.gpsimd, "sp": nc.sync}
        self.semh = {}
        self.nep = {k: 0 for k in self.engs}
        self.cnt = {k: 0 for k in self.engs}
        for k in self.engs:
            self._new_epoch(k)
        self.waited = {k: {} for k in self.engs}
        self.strict = set(strict)
        self.ndma = ndma
        for i in range(ndma):
            self.semh[("d", i)] = es.enter_context(nc.semaphore(f"dq{i}"))
        self.dval = [0] * ndma
        self.dpool = {"sp": list(range(0, ndma // 2)), "pool": list(range(ndma // 2, ndma))}
        self.dnext = {"sp": 0, "pool": 0}
        self.bufs = {}
        self.nwaits = 0
        self.nops = {k: 0 for k in self.engs}

    def _new_epoch(self, k):
        ep = self.nep[k]
        self.semh[("e", k, ep)] = self.es.enter_context(self.nc.semaphore(f"s_{k}{ep}"))
        self.nep[k] = ep + 1
        self.cnt[k] = 0

    def _buf(self, key):
        b = self.bufs.get(key)
        if b is None:
            b = {"w": {}, "r": {}}
            self.bufs[key] = b
        return b

    def _wait(self, eng, tok):
        semkey, val = tok
        if semkey[0] == "e":
            if semkey[1] == eng and eng not in self.strict:
                return
            w = self.waited[eng]
            for ep in range(semkey[2] + 1, self.nep[semkey[1]]):
                if ("e", semkey[1], ep) in w:
                    return
        else:
            w = self.waited[eng]
        if w.get(semkey, 0) >= val:
            return
        self.engs[eng].wait_ge(self.semh[semkey], val)
        w[semkey] = val
        self.nwaits += 1

    def _deps(self, eng, reads, writes):
        toks = {}
        for k in reads:
            b = self._buf(k)
            for src, t in b["w"].items():
                toks[(src, t)] = t
        for k in writes:
            b = self._buf(k)
            for src, t in b["w"].items():
                toks[(src, t)] = t
            for src, t in b["r"].items():
                toks[(src, t)] = t
        for t in toks.values():
            self._wait(eng, t)

    def op(self, eng, fn, reads=(), writes=()):
        self._deps(eng, reads, writes)
        ins = fn()
        self.cnt[eng] += 1
        self.nops[eng] += 1
        ep = self.nep[eng] - 1
        semkey = ("e", eng, ep)
        ins.then_inc(self.semh[semkey], 1)
        tok = (semkey, self.cnt[eng])
        if self.cnt[eng] >= self.LIMIT:
            self._new_epoch(eng)
        src = ("e", eng)
        for k in writes:
            self._buf(k)["w"][src] = tok
        for k in reads:
            self._buf(k)["r"][src] = tok
        return tok

    def dma(self, q, out, in_, reads=(), writes=()):
        self._deps(q, reads, writes)
        pl = self.dpool[q]
        i = pl[self.dnext[q]]
        self.dnext[q] = (self.dnext[q] + 1) % len(pl)
        semkey = ("d", i)
        if self.dval[i] > 0:
            self._wait(q, (semkey, self.dval[i]))
        assert self.dval[i] + 16 < 65000
        ins = self.engs[q].dma_start(out=out, in_=in_)
        self.dval[i] += 16
        ins.then_inc(self.semh[semkey], 16)
        tok = (semkey, self.dval[i])
        for k in writes:
            self._buf(k)["w"][semkey] = tok
        for k in reads:
            self._buf(k)["r"][semkey] = tok
        return tok

    def idma(self, out, in_, idx_ap, reads=(), writes=()):
        q = "pool"
        if os.environ.get("NOIDMA"):
            return self.dma(q, out, in_[0:128, :], reads=reads, writes=writes)
        self._deps(q, reads, writes)
        pl = self.dpool[q]
        i = pl[self.dnext[q]]
        self.dnext[q] = (self.dnext[q] + 1) % len(pl)
        semkey = ("d", i)
        if self.dval[i] > 0:
            self._wait(q, (semkey, self.dval[i]))
        assert self.dval[i] + 16 < 65000
        ins = self.nc.gpsimd.indirect_dma_start(out=out, out_offset=None, in_=in_,
                                                in_offset=bass.IndirectOffsetOnAxis(ap=idx_ap, axis=0))
        self.dval[i] += 16
        ins.then_inc(self.semh[semkey], 16)
        tok = (semkey, self.dval[i])
        for k in writes:
            self._buf(k)["w"][semkey] = tok
        for k in reads:
            self._buf(k)["r"][semkey] = tok
        return tok

    def barrier(self):
        toks = []
        for k in self.engs:
            if self.cnt[k] > 0:
                toks.append((("e", k, self.nep[k] - 1), self.cnt[k]))
            elif self.nep[k] > 1:
                toks.append((("e", k, self.nep[k] - 2), self.LIMIT))
        for i in range(self.ndma):
            if self.dval[i] > 0:
                toks.append((("d", i), self.dval[i]))
        for k in self.engs:
            st = self.strict
            self.strict = set(self.engs)
            for t in toks:
                if t[0][0] == "e" and t[0][1] == k:
                    continue
                self._wait(k, t)
            self.strict = st

    def finish(self):
        for i in range(self.ndma):
            if self.dval[i] > 0:
                self._wait("sp", (("d", i), self.dval[i]))
        for k in self.engs:
            if k == "sp":
                continue
            if self.cnt[k] > 0:
                self._wait("sp", (("e", k, self.nep[k] - 1), self.cnt[k]))


class Prog:
    def __init__(self, mode, stage=99):
        self.stage = stage
        self.mode = mode
        self.nc = bass.Bass("TRN2", target_bir_lowering=False)
        self.es = ExitStack()

    def dram_in(self, name, shape, dt=F32):
        return self.nc.dram_tensor(name, list(shape), dt, kind="ExternalInput").ap()

    def dram_out(self, name, shape, dt=F32):
        return self.nc.dram_tensor(name, list(shape), dt, kind="ExternalOutput").ap()

    def sb(self, name, shape, dt, es=None):
        return (es or self.es).enter_context(self.nc.sbuf_tensor("sb_" + name, list(shape), dt))

    def ps(self, name, shape, dt=F32, es=None):
        return (es or self.es).enter_context(self.nc.psum_tensor("ps_" + name, list(shape), dt))

    def build(self):
        with self.es:
            self.sch = Sched(self.nc, self.es)
            self._build_F()
            self.sch.finish()
        return self.nc

    def _common_alloc(self, ntiles):
        nc = self.nc
        self.ntiles = ntiles
        self.x = self.sb("x", [128, ntiles, D], F32)
        self.xT = self.sb("xT", [128, KC, ntiles * 128], BF16)
        self.arena = self.sb("arena", [128, 6, 4096], BF16)
        self.lnb = self.sb("lnb", [128, 2, D], F32)
        self.gates = self.sb("gates", [128, ntiles, NE], F32)
        self.ident = self.sb("ident", [128, 128], F32)
        self.rw = self.sb("rw", [128, KC, NE], F32)
        self.rbias = self.sb("rbias", [128, NE], F32)
        self.consts = self.sb("consts", [128, 8], F32)
        self.stat = self.sb("stat", [128, 2, 6], F32)
        self.mv = self.sb("mv", [128, 8], F32)
        self.xTf = self.sb("xTf", [128, KC, 128], F32)
        self.rt = self.sb("rt", [128, 8, NE], F32)
        self.pp = [self.ps(f"pp{i}", [128, 1024], F32) for i in range(4)]
        self.d_ident = self.dram_in("ident", [128, 128])
        self.d_rw = self.dram_in("rw", [128, KC * NE])
        self.d_rbias = self.dram_in("rbias", [128, NE])
        self.d_lnb = self.dram_in("lnb", [DEPTH, 4, 128, D])
        nl = getattr(self, "n_moe_layers", DEPTH)
        ne_decl = NE if nl > 0 else 1
        nl = max(nl, 1)
        self.d_wg = self.dram_in("moe_w_gate", [nl, ne_decl, D, DEXP])
        self.d_wu = self.dram_in("moe_w_up", [nl, ne_decl, D, DEXP])
        self.d_wd = self.dram_in("moe_w_down", [nl, ne_decl, DEXP, D])
        s = self.sch
        s.dma("sp", self.ident[:], self.d_ident[:, :], writes=["ident"])
        s.dma("sp", self.rw[:].rearrange("p k e -> p (k e)"), self.d_rw[:, :], writes=["rw"])
        s.dma("sp", self.rbias[:], self.d_rbias[:, :], writes=["rbias"])
        s.op("pool", lambda: nc.gpsimd.memset(self.consts[:, 0:1], -0.5), writes=["consts"])
        s.op("pool", lambda: nc.gpsimd.memset(self.consts[:, 1:2], LN_EPS), writes=["consts"])
        s.op("pool", lambda: nc.gpsimd.memset(self.consts[:, 2:3], LN_EPS / (ALPHA * ALPHA)), writes=["consts"])
        self.wslot = 0

    def slot_gu(self, i):
        return self.arena[:, i, :].rearrange("p (k n) -> p k n", n=DEXP)

    def slot_d(self, i):
        return self.arena[:, i, :].rearrange("p (k n) -> p k n", n=D)

    def rsqrt_dve(self, out, in_, tmp, rkeys, okey, tkey):
        nc, s = self.nc, self.sch
        I32 = mybir.dt.int32
        s.op("dve", lambda: nc.vector.tensor_scalar(out.bitcast(I32), in_.bitcast(I32), 1, None, ALU.arith_shift_right),
             reads=rkeys, writes=[okey])
        s.op("dve", lambda: nc.vector.tensor_scalar(out.bitcast(I32), out.bitcast(I32), -1.0, 1597463007.0, ALU.mult, ALU.add),
             reads=[okey], writes=[okey])
        for _ in range(3):
            s.op("dve", lambda: nc.vector.tensor_tensor(tmp, out, out, ALU.mult), reads=[okey], writes=[tkey])
            s.op("dve", lambda: nc.vector.tensor_tensor(tmp, tmp, in_, ALU.mult), reads=[tkey] + rkeys, writes=[tkey])
            s.op("dve", lambda: nc.vector.tensor_scalar(tmp, tmp, -0.5, 1.5, ALU.mult, ALU.add), reads=[tkey], writes=[tkey])
            s.op("dve", lambda: nc.vector.tensor_tensor(out, out, tmp, ALU.mult), reads=[okey, tkey], writes=[okey])

    def ln_tile(self, t, src_key, eps_col, do_xT=True, do_router=True, last=False):
        nc, s = self.nc, self.sch
        xt = self.x[:, t, :]
        xk = ("x", t)
        for hh in range(2):
            s.op("dve", lambda hh=hh: nc.vector.bn_stats(self.stat[:, hh, :], self.x[:, t, hh * 512:(hh + 1) * 512]),
                 reads=[xk], writes=["stat"])
        s.op("dve", lambda: nc.vector.bn_aggr(self.mv[:, 0:2], self.stat[:].rearrange("p a b -> p (a b)")),
             reads=["stat"], writes=["mv"])
        s.op("dve", lambda: nc.vector.tensor_scalar(self.mv[:, 2:3], self.mv[:, 1:2], self.consts[:, eps_col:eps_col + 1], None, ALU.add),
             reads=["mv", "consts"], writes=["mv2"])
        self.rsqrt_dve(self.mv[:, 3:4], self.mv[:, 2:3], self.mv[:, 4:5], ["mv2"], "mv3", "mv4")
        s.op("dve", lambda: nc.vector.tensor_scalar(xt, xt, self.mv[:, 0:1], self.mv[:, 3:4], ALU.subtract, ALU.mult),
             reads=[xk, "mv", "mv3"], writes=[xk])
        s.op("dve", lambda: nc.vector.tensor_tensor(xt, xt, self.lnb[:, 0, :], ALU.mult),
             reads=[xk, "lnb"], writes=[xk])
        s.op("dve", lambda: nc.vector.tensor_tensor(xt, xt, self.lnb[:, 1, :], ALU.add),
             reads=[xk, "lnb"], writes=[xk])
        if not do_xT:
            return
        pt = self.pp[3]
        for c in range(KC):
            s.op("pe", lambda c=c: nc.tensor.transpose(pt[:, c * 128:(c + 1) * 128], self.x[:, t, c * 128:(c + 1) * 128], self.ident[:]),
                 reads=[xk, "ident"], writes=[("pp", 3, c // 4)])
        xTk = ("xT", t)
        if not do_router:
            s.op("act", lambda: nc.scalar.copy(self.xT[:, :, t * 128:(t + 1) * 128], pt[:].rearrange("p (c n) -> p c n", n=128)),
                 reads=[("pp", 3, 0), ("pp", 3, 1)], writes=[xTk])
            return
        s.op("act", lambda: nc.scalar.copy(self.xTf[:], pt[:].rearrange("p (c n) -> p c n", n=128)),
             reads=[("pp", 3, 0), ("pp", 3, 1)], writes=["xTf"])
        s.op("dve", lambda: nc.vector.tensor_copy(self.xT[:, :, t * 128:(t + 1) * 128], self.xTf[:]),
             reads=["xTf"], writes=[xTk])
        if RCUT <= 1:
            return
        pr = self.pp[2]
        for c in range(KC):
            s.op("pe", lambda c=c: nc.tensor.matmul(pr[:, 0:NE], self.xTf[:, c, :], self.rw[:, c, :], start=(c == 0), stop=(c == KC - 1)),
                 reads=["xTf", "rw"], writes=[("pp", 2, 0)])
        if RCUT <= 2:
            return
        self.router_gates(t, pr[:, 0:NE], ("pp", 2, 0))

    def router_gates(self, t, logits_ps, pkey):
        nc, s = self.nc, self.sch
        rt = self.rt
        aff, sel, tmp, eq, msk = rt[:, 0, :], rt[:, 1, :], rt[:, 2, :], rt[:, 3, :], rt[:, 4, :]
        m1, m2, gs, gsel = rt[:, 5, 0:4], rt[:, 5, 4:8], rt[:, 5, 8:12], rt[:, 5, 12:16]
        gmax, wsum = rt[:, 6, 0:1], rt[:, 6, 1:2]
        R = ["rt"]
        g3 = lambda a: a.rearrange("p (g e) -> p g e", e=4)
        s.op("act", lambda: nc.scalar.activation(out=aff, in_=logits_ps, func=AF.Sigmoid), reads=[pkey], writes=R)
        s.op("dve", lambda: nc.vector.tensor_tensor(sel, aff, self.rbias[:], ALU.add), reads=R + ["rbias"], writes=R)
        s.op("dve", lambda: nc.vector.tensor_reduce(m1, g3(sel), AX.X, ALU.max), reads=R, writes=R)
        s.op("dve", lambda: nc.vector.tensor_tensor(g3(eq), g3(sel), self._b3(m1), ALU.is_equal), reads=R, writes=R)
        s.op("dve", lambda: nc.vector.scalar_tensor_tensor(tmp, eq, -1.0e9, sel, ALU.mult, ALU.add), reads=R, writes=R)
        s.op("dve", lambda: nc.vector.tensor_reduce(m2, g3(tmp), AX.X, ALU.max), reads=R, writes=R)
        s.op("dve", lambda: nc.vector.tensor_tensor(gs, m1, m2, ALU.add), reads=R, writes=R)
        s.op("dve", lambda: nc.vector.tensor_reduce(gmax, gs, AX.X, ALU.max), reads=R, writes=R)
        s.op("dve", lambda: nc.vector.tensor_scalar(gsel, gs, gmax, None, ALU.is_equal), reads=R, writes=R)
        s.op("dve", lambda: nc.vector.tensor_tensor(g3(msk), g3(sel), self._b3(m2), ALU.is_ge), reads=R, writes=R)
        s.op("dve", lambda: nc.vector.tensor_tensor(g3(msk), g3(msk), self._b3(gsel), ALU.mult), reads=R, writes=R)
        s.op("dve", lambda: nc.vector.tensor_tensor(tmp, aff, msk, ALU.mult), reads=R, writes=R)
        s.op("dve", lambda: nc.vector.tensor_reduce(wsum, tmp, AX.X, ALU.add), reads=R, writes=R)
        s.op("dve", lambda: nc.vector.reciprocal(wsum, wsum), reads=R, writes=R)
        s.op("dve", lambda: nc.vector.tensor_scalar(self.gates[:, t, :], tmp, wsum, 1.0 / ALPHA, ALU.mult, ALU.mult),
             reads=R, writes=[("gates", t)])

    def _b3(self, a):
        return a.unsqueeze(2).to_broadcast([128, 4, 4])

    def moe(self, layer, chunks, es, tail=None):
        nc, s = self.nc, self.sch
        sg = self.sb(f"moe_s{layer}", [128, 2, 512], BF16, es)
        hT = self.sb(f"moe_h{layer}", [128, 2, 4, 512], BF16, es)
        it = 0
        for e in range(NE):
            sl = []
            for j, (dw, nk) in enumerate(((self.d_wg, KC), (self.d_wu, KC), (self.d_wd, 4))):
                i = self.wslot
                self.wslot = (self.wslot + 1) % 6
                dst = self.slot_gu(i) if j < 2 else self.slot_d(i)
                s.dma("pool", dst, dw[layer, e].rearrange("(k p) n -> p k n", p=128), writes=[("arena", i)])
                sl.append(i)
            wg, wu, wd = self.slot_gu(sl[0]), self.slot_gu(sl[1]), self.slot_d(sl[2])
            for ch in chunks:
                t0 = ch[0]
                tok = slice(t0 * 128, t0 * 128 + 512)
                hb = it % 2
                for j in range(4):
                    pb = self.pp[(it * 4 + j) % 2]
                    pg, pu = pb[:, 0:512], pb[:, 512:1024]
                    kg, ku = ("pp", (it * 4 + j) % 2, 0), ("pp", (it * 4 + j) % 2, 1)
                    xk = [("xT", t) for t in ch]
                    for k in range(KC):
                        s.op("pe", lambda k=k, j=j, pg=pg: nc.tensor.matmul(pg, wg[:, k, j * 128:(j + 1) * 128], self.xT[:, k, tok], start=(k == 0), stop=(k == KC - 1)),
                             reads=xk + [("arena", sl[0])], writes=[kg])
                    for k in range(KC):
                        s.op("pe", lambda k=k, j=j, pu=pu: nc.tensor.matmul(pu, wu[:, k, j * 128:(j + 1) * 128], self.xT[:, k, tok], start=(k == 0), stop=(k == KC - 1)),
                             reads=xk + [("arena", sl[1])], writes=[ku])
                    sb_ = (it * 4 + j) % 2
                    s.op("act", lambda pg=pg, sb_=sb_: nc.scalar.activation(out=sg[:, sb_, :], in_=pg, func=AF.Silu),
                         reads=[kg], writes=[("moe_s", sb_)])
                    s.op("dve", lambda pu=pu, sb_=sb_, j=j, hb=hb: nc.vector.tensor_tensor(hT[:, hb, j, :], pu, sg[:, sb_, :], ALU.mult),
                         reads=[ku, ("moe_s", sb_)], writes=[("moe_h", hb)])
                for tt, t in enumerate(ch):
                    py = self.pp[2 + (it * 4 + tt) % 2]
                    yk = [("pp", 2 + (it * 4 + tt) % 2, 0), ("pp", 2 + (it * 4 + tt) % 2, 1)]
                    for hh in range(2):
                        for j in range(4):
                            s.op("pe", lambda hh=hh, j=j, tt=tt, py=py, hb=hb: nc.tensor.matmul(py[:, hh * 512:(hh + 1) * 512], hT[:, hb, j, tt * 128:(tt + 1) * 128], wd[:, j, hh * 512:(hh + 1) * 512], start=(j == 0), stop=(j == 3)),
                                 reads=[("moe_h", hb), ("arena", sl[2])], writes=[yk[hh]])
                    s.op("dve", lambda t=t, py=py: nc.vector.scalar_tensor_tensor(self.x[:, t, :], py[:], self.gates[:, t, e:e + 1], self.x[:, t, :], ALU.mult, ALU.add),
                         reads=yk + [("gates", t), ("x", t)], writes=[("x", t)])
                it += 1
                if tail is not None and e == NE - 1:
                    tail(ch)

    def load_lnb(self, layer, which):
        s = self.sch
        s.dma("sp", self.lnb[:, 0, :], self.d_lnb[layer, 2 * which], writes=["lnb", "oS"])
        s.dma("sp", self.lnb[:, 1, :], self.d_lnb[layer, 2 * which + 1], writes=["lnb", "tmpf2"])

    def _build_F(self):
        nc = self.nc
        self.n_moe_layers = DEPTH
        self._common_alloc(NT_MAIN + NT_HALO)
        s = self.sch
        d_x = self.dram_in("xin", [(NT_MAIN + NT_HALO) * 128, D])
        d_hv = self.dram_in("halo_valid", [128, NPIECE])
        d_pw1 = self.dram_in("a_w_pw1", [N_A, D, 2 * D])
        d_pw2 = self.dram_in("a_w_pw2", [N_A, D, D])
        d_cfm = self.dram_in("cfm", [N_A, 128, 288])
        d_bpw2 = self.dram_in("a_b_pw2", [N_A, D])
        d_wkv = self.dram_in("w_kv", [D, 2 * D])
        d_ub = self.dram_in("ubias", [128, self.NUNIT])
        self.d_gt = self.dram_in("gtab", [NH, 128, 1024])
        d_b31 = self.dram_in("b31r", [128, NH])
        self.d_wq = self.dram_in("b_w_q", [2, D, D])
        self.d_wo = self.dram_in("b_w_o", [2, D, D])
        d_lam = self.dram_in("b_lambda", [2, 1, 256])
        d_sg = self.dram_in("sublng", [128, 2])
        self.d_idx = self.dram_in("kvidx", [NH, 128, self.NUNIT], mybir.dt.int32)
        o_x = self.dram_out("x_out", [NT_MAIN * 128, D])
        self.kvsrc = nc.dram_tensor("kvsrc", [KVROWS, 1024], BF16, kind="Internal").ap()
        self.kvall = nc.dram_tensor("kvall", [4 * KVROWS, 1024], BF16, kind="Internal").ap()
        self.cc_sem = self.es.enter_context(nc.semaphore("cc_sem"))
        self.w1s = nc.dram_tensor("w1s", [N_A, KC, 128, KC * 256], BF16, kind="Internal").ap()
        for layer in range(N_A):
            for c in range(KC):
                dst = self.w1s[layer, c].rearrange("p (k n) -> p k n", n=256)
                s.dma("pool", dst[:, :, 0:128], d_pw1[layer, :, c * 128:(c + 1) * 128].rearrange("(k p) n -> p k n", p=128), writes=[("w1s", layer, c)])
                s.dma("pool", dst[:, :, 128:256], d_pw1[layer, :, D + c * 128:D + (c + 1) * 128].rearrange("(k p) n -> p k n", p=128), writes=[("w1s", layer, c)])

        with ExitStack() as esA:
            hv = self.sb("hv", [128, NPIECE], F32, esA)
            cfm = self.sb("cfm", [128, 288], F32, esA)
            ghist = self.sb("ghist", [128, NPIECE, KC, 30], F32, esA)
            browf = self.sb("browf", [1, D], F32, esA)
            ones1 = self.sb("ones1", [1, 128], F32, esA)
            onesm = self.sb("onesm", [128, 128], F32, esA)
            s.dma("sp", hv[:], d_hv[:, :], writes=["hv"])
            s.op("pool", lambda: nc.gpsimd.memset(ones1[:], 1.0), writes=["ones1"])
            s.op("pool", lambda: nc.gpsimd.memset(onesm[:], 1.0 / D), writes=["onesm"])
            for t in range(self.ntiles):
                s.dma("sp", self.x[:, t, :], d_x[t * 128:(t + 1) * 128, :], writes=[("x", t)])
            for t in range(self.ntiles):
                self.transpose_only(t)
            for layer in range(N_A):
                with ExitStack() as es:
                    self.conformer_layer(layer, es, d_pw1, d_pw2, d_cfm, d_bpw2, hv, cfm, ghist, None, browf, ones1, onesm)
                s.barrier()
                with ExitStack() as es:
                    nch = 5 if layer == 0 else 4
                    chunks = [[4 * c + i for i in range(4)] for c in range(nch)]
                    self.load_lnb(layer, 1)
                    self.moe(layer, chunks, es, tail=lambda ch: [self.ln_tile(t, None, 2, do_xT=True, do_router=False) for t in ch])
                s.barrier()
            with ExitStack() as es:
                self.kv_proj(es, d_wkv, None, None)
            s.barrier()

        with ExitStack() as esB:
            self.qT0 = self.sb("qT0", [128, NH, 512], BF16, esB)
            self.ub = self.sb("ub", [128, self.NUNIT], F32, esB)
            self.fb = self.sb("fb", [128, self.NUNIT], F32, esB)
            self.b31 = self.sb("b31", [128, NH], F32, esB)
            self.lamt = self.sb("lamt", [1, 256], F32, esB)
            self.lamw = self.sb("lamw", [1, 8], F32, esB)
            self.sgl = self.sb("sgl", [128, 4], F32, esB)
            self.ones1f = self.sb("ones1f", [1, 128], F32, esB)
            self.onesv = self.sb("onesv", [128, 128], F32, esB)
            self.onesb = self.sb("onesb", [128, 128], BF16, esB)
            self.lamb = self.sb("lamb", [128, 1], F32, esB)
            self.idxb = self.sb("idxb", [128, NH, self.NUNIT], mybir.dt.int32, esB)
            s.dma("sp", self.idxb[:], self.d_idx.rearrange("h p u -> p h u"), writes=["idx"])
            s.dma("sp", self.ub[:], d_ub[:, :], writes=["ub"])
            s.dma("sp", self.b31[:], d_b31[:, :], writes=["b31"])
            s.dma("sp", self.sgl[:, 0:2], d_sg[:, :], writes=["sgl"])
            s.op("pool", lambda: nc.gpsimd.memset(self.ones1f[:], 1.0), writes=["ones1f"])
            s.op("pool", lambda: nc.gpsimd.memset(self.onesv[:], 1.0 / VD), writes=["onesv"])
            s.op("pool", lambda: nc.gpsimd.memset(self.onesb[:], 1.0), writes=["onesb"])
            for j in range(2):
                layer = N_A + j
                s.dma("sp", self.lamt[:], d_lam[j], writes=["lamt"])
                self.attention_layer(layer, j)
                s.barrier()
                with ExitStack() as es:
                    chunks = [[4 * c + i for i in range(4)] for c in range(4)]
                    self.load_lnb(layer, 1)
                    self.moe(layer, chunks, es, tail=lambda ch, j=j: [self.ln_tile(t, None, 2, do_xT=(j == 0), do_router=False) for t in ch])
                s.barrier()
            for t in range(NT_MAIN):
                s.dma("sp", o_x[t * 128:(t + 1) * 128, :], self.x[:, t, :], reads=[("x", t)])

    def kv_exchange(self):
        self.nc.gpsimd.wait_ge(self.cc_sem, KVSPLIT)

    def qap(self, h, sl):
        if sl == 0:
            return self.qT0[:, h, :]
        if sl == 3:
            return self.xT[:, h, NT_MAIN * 128:NT_MAIN * 128 + 512]
        t = NT_MAIN + 2 * (sl - 1) + h // 4
        return self.x[:, t, :].bitcast(BF16)[:, (h % 4) * 512:(h % 4 + 1) * 512]

    def transpose_only(self, t):
        nc, s = self.nc, self.sch
        pt = self.pp[3]
        xk = ("x", t)
        for c in range(KC):
            s.op("pe", lambda c=c: nc.tensor.transpose(pt[:, c * 128:(c + 1) * 128], self.x[:, t, c * 128:(c + 1) * 128], self.ident[:]),
                 reads=[xk, "ident"], writes=[("pp", 3, c // 4)])
        s.op("act", lambda: nc.scalar.copy(self.xT[:, :, t * 128:(t + 1) * 128], pt[:].rearrange("p (c n) -> p c n", n=128)),
             reads=[("pp", 3, 0), ("pp", 3, 1)], writes=[("xT", t)])

    def conformer_layer(self, layer, es, d_pw1, d_pw2, d_cfm, d_bpw2, hv, cfm, ghist, brow, browf, ones1, onesm):
        nc, s = self.nc, self.sch
        NS = 256
        w2 = self.arena[:, 0:2, :].rearrange("p s (k n) -> p (s k) n", n=D)
        hn = self.arena[:, 2, :].rearrange("p (k n) -> p k n", n=512)[:, :, 0:NS]
        w1buf = self.arena[:, 3:5, :].rearrange("p s (b k n) -> p (s b) k n", b=2, k=KC)
        scr = self.arena[:, 5, :].bitcast(F32)
        hB = scr[:, 0:NS]
        sq = [scr[:, NS:2 * NS], scr[:, 2 * NS:3 * NS]]
        tps = [scr[:, (3 + i) * NS:(4 + i) * NS] for i in range(4)]
        NT_ACT = 15
        self.tapi = 0
        s.dma("pool", w2, d_pw2[layer].rearrange("(k p) n -> p k n", p=128), writes=[("arena", 0), ("arena", 1)])
        s.dma("sp", cfm[:], d_cfm[layer], writes=["cfm"])
        s.dma("sp", browf[:], d_bpw2[layer:layer + 1, :], writes=["browf"])
        self.load_lnb(layer, 0)
        b1 = cfm[:, 0:16]
        wdw = cfm[:, 16:16 + 248].rearrange("p (c j) -> p c j", j=CONVW)
        bdw = cfm[:, 264:272]
        lng = cfm[:, 272:280]
        lnbt = cfm[:, 280:288]

        gbuf = self.sb(f"gbuf{layer}", [128, 2, 30 + NS], F32, es)
        h = self.sb(f"h{layer}", [128, KC, NS], F32, es)
        sgt = self.sb(f"sgt{layer}", [128, 2, NS], F32, es)
        mean = self.sb(f"mean{layer}", [128, NS], F32, es)
        rstd = self.sb(f"rstd{layer}", [128, NS], F32, es)

        subs = []
        for i in range(2):
            subs.append(("halo", [16 + 2 * i, 17 + 2 * i]))
        for p in range(NPIECE):
            for i in range(2):
                subs.append((("main", p, i), [4 * p + 2 * i, 4 * p + 2 * i + 1]))
        self.w1i = 0
        for kind, tiles in subs:
            full = (kind != "halo") or (layer == 0)
            tok = slice(tiles[0] * 128, tiles[0] * 128 + NS)
            xk = [("xT", t) for t in tiles]
            def mm(c, kind=kind, tiles=tiles, tok=tok, xk=xk):
                wb = self.w1i % 4
                self.w1i += 1
                wt = w1buf[:, wb]
                s.dma("sp", wt.rearrange("p k n -> p (k n)"), self.w1s[layer, c], reads=[("w1s", layer, c)], writes=[("w1", wb)])
                pb = self.pp[c % 2]
                pa, pg = pb[:, 0:NS], pb[:, 512:512 + NS]
                ka, kg = ("pp", c % 2, 0), ("pp", c % 2, 1)
                for k in range(KC):
                    s.op("pe", lambda k=k: nc.tensor.matmul(pa, wt[:, k, 0:128], self.xT[:, k, tok], start=(k == 0), stop=(k == KC - 1)),
                         reads=xk + [("w1", wb)], writes=[ka])
                for k in range(KC):
                    s.op("pe", lambda k=k: nc.tensor.matmul(pg, wt[:, k, 128:256], self.xT[:, k, tok], start=(k == 0), stop=(k == KC - 1)),
                         reads=xk + [("w1", wb)], writes=[kg])

            def glu(c, kind=kind, tiles=tiles):
                pb = self.pp[c % 2]
                pa, pg = pb[:, 0:NS], pb[:, 512:512 + NS]
                ka, kg = ("pp", c % 2, 0), ("pp", c % 2, 1)
                gb = c % 2
                gk = ("gbuf", gb)
                s.op("act", lambda: nc.scalar.activation(out=sgt[:, gb, :], in_=pg, func=AF.Sigmoid, bias=b1[:, 8 + c:9 + c]),
                     reads=[kg, "cfm"], writes=[("sgt", gb)])
                if kind == "halo":
                    s.op("dve", lambda: nc.vector.memset(gbuf[:, gb, 0:30], 0.0), writes=[gk])
                else:
                    p = kind[1]
                    s.op("act", lambda: nc.scalar.copy(gbuf[:, gb, 0:30], ghist[:, p, c, :]),
                         reads=[("ghist", p)], writes=[gk])
                s.op("dve", lambda: nc.vector.scalar_tensor_tensor(gbuf[:, gb, 30:30 + NS], pa, b1[:, c:c + 1], sgt[:, gb, :], ALU.add, ALU.mult),
                     reads=[ka, ("sgt", gb), "cfm"], writes=[gk])
                if kind == "halo":
                    for i, t in enumerate(tiles):
                        p = t - 16
                        s.op("dve", lambda p=p, i=i: nc.vector.tensor_scalar(ghist[:, p, c, :], gbuf[:, gb, 30 + 128 * i + 98:30 + 128 * (i + 1)], hv[:, p:p + 1], None, ALU.mult),
                             reads=[gk, "hv"], writes=[("ghist", p)])
                elif kind[2] == 0:
                    p = kind[1]
                    s.op("act", lambda: nc.scalar.copy(ghist[:, p, c, :], gbuf[:, gb, NS:NS + 30]),
                         reads=[gk], writes=[("ghist", p)])

            def conv(c):
                gb = c % 2
                gk = ("gbuf", gb)
                hk = ("h", c)
                pacc = self.pp[3][:, (c % 2) * 512:(c % 2) * 512 + NS]
                pk3 = ("pp", 3, c % 2)
                act_taps = list(range(CONVW - NT_ACT, CONVW))
                for i, j in enumerate(act_taps):
                    tb = self.tapi % 4
                    self.tapi += 1
                    tbuf, tkey = tps[tb], ("tap", tb)
                    s.op("act", lambda j=j, tbuf=tbuf: nc.scalar.activation(out=tbuf, in_=gbuf[:, gb, j:j + NS], func=AF.Copy, scale=wdw[:, c, j:j + 1]),
                         reads=[gk, "cfm"], writes=[tkey])
                    s.op("pe", lambda i=i, tbuf=tbuf: nc.tensor.matmul(pacc, self.ident[:], tbuf, start=(i == 0), stop=(i == len(act_taps) - 1)),
                         reads=[tkey, "ident"], writes=[pk3])
                s.op("dve", lambda: nc.vector.tensor_scalar(h[:, c, :], gbuf[:, gb, 0:NS], wdw[:, c, 0:1], bdw[:, c:c + 1], ALU.mult, ALU.add),
                     reads=[gk, "cfm"], writes=[hk])
                s.op("dve", lambda: nc.vector.tensor_scalar(hB, gbuf[:, gb, 1:1 + NS], wdw[:, c, 1:2], None, ALU.mult),
                     reads=[gk, "cfm"], writes=["hB"])
                for j in range(2, CONVW - NT_ACT):
                    acc, ak = (h[:, c, :], hk) if j % 2 == 0 else (hB, "hB")
                    s.op("dve", lambda j=j, acc=acc: nc.vector.scalar_tensor_tensor(acc, gbuf[:, gb, j:j + NS], wdw[:, c, j:j + 1], acc, ALU.mult, ALU.add),
                         reads=[gk, "cfm", ak], writes=[ak])
                s.op("dve", lambda: nc.vector.tensor_tensor(h[:, c, :], h[:, c, :], hB, ALU.add), reads=[hk, "hB"], writes=[hk])
                s.op("dve", lambda: nc.vector.tensor_tensor(h[:, c, :], h[:, c, :], pacc, ALU.add), reads=[hk, pk3], writes=[hk])

            def stats(c):
                s.op("act", lambda: nc.scalar.activation(out=sq[c % 2], in_=h[:, c, :], func=AF.Square),
                     reads=[("h", c)], writes=[("sq", c % 2)])
                s.op("pe", lambda: nc.tensor.matmul(self.pp[2][:, 0:NS], onesm[:], h[:, c, :], start=(c == 0), stop=(c == KC - 1)),
                     reads=[("h", c), "onesm"], writes=[("pp", 2, 0)])
                s.op("pe", lambda: nc.tensor.matmul(self.pp[2][:, 512:512 + NS], onesm[:], sq[c % 2], start=(c == 0), stop=(c == KC - 1)),
                     reads=[("sq", c % 2), "onesm"], writes=[("pp", 2, 1)])

            mm(0)
            mm(1)
            glu(0)
            for c in range(KC):
                if c + 2 < KC:
                    mm(c + 2)
                if c + 1 < KC:
                    glu(c + 1)
                if full:
                    if c > 0:
                        stats(c - 1)
                    conv(c)
            if full:
                stats(KC - 1)
            if not full or CUT <= 3:
                continue
            s.op("act", lambda: nc.scalar.copy(mean[:], self.pp[2][:, 0:NS]), reads=[("pp", 2, 0)], writes=["mean"])
            s.op("act", lambda: nc.scalar.activation(out=rstd[:], in_=self.pp[2][:, 0:NS], func=AF.Square), reads=[("pp", 2, 0)], writes=["rstd"])
            s.op("dve", lambda: nc.vector.scalar_tensor_tensor(rstd[:], rstd[:], -1.0, self.pp[2][:, 512:512 + NS], ALU.mult, ALU.add),
                 reads=["rstd", ("pp", 2, 1)], writes=["rstd"])
            s.op("dve", lambda: nc.vector.tensor_scalar(sgt[:, 1, :], rstd[:], LN_EPS, None, ALU.add), reads=["rstd"], writes=[("sgt", 1)])
            self.rsqrt_dve(rstd[:], sgt[:, 1, :], sgt[:, 0, :], [("sgt", 1)], "rstd", ("sgt", 0))
            for c in range(KC):
                hk = ("h", c)
                s.op("dve", lambda c=c: nc.vector.tensor_tensor(h[:, c, :], h[:, c, :], mean[:], ALU.subtract), reads=[hk, "mean"], writes=[hk])
                s.op("dve", lambda c=c: nc.vector.tensor_tensor(h[:, c, :], h[:, c, :], rstd[:], ALU.mult), reads=[hk, "rstd"], writes=[hk])
                s.op("act", lambda c=c: nc.scalar.activation(out=hn[:, c, :], in_=h[:, c, :], func=AF.Silu, bias=lnbt[:, c:c + 1], scale=lng[:, c:c + 1]),
                     reads=[hk, "cfm"], writes=["hn", ("arena", 2)])
            if CUT <= 4:
                continue
            for i, t in enumerate(tiles):
                py = self.pp[3] if False else self.pp[2 + (i % 2)]
                pidx = 2 + (i % 2)
                yk = [("pp", pidx, 0), ("pp", pidx, 1)]
                for hh in range(2):
                    for c in range(KC):
                        s.op("pe", lambda c=c, hh=hh, py=py, i=i: nc.tensor.matmul(py[:, hh * 512:(hh + 1) * 512], hn[:, c, i * 128:(i + 1) * 128], w2[:, c, hh * 512:(hh + 1) * 512], start=(c == 0), stop=False),
                             reads=["hn", ("arena", 0), ("arena", 1)], writes=[yk[hh]])
                    s.op("pe", lambda hh=hh, py=py: nc.tensor.matmul(py[:, hh * 512:(hh + 1) * 512], ones1[:], browf[:, hh * 512:(hh + 1) * 512], start=False, stop=True),
                         reads=["ones1", "browf"], writes=[yk[hh]])
                s.op("dve", lambda t=t, py=py: nc.vector.scalar_tensor_tensor(self.x[:, t, :], self.x[:, t, :], ALPHA, py[:], ALU.mult, ALU.add),
                     reads=yk + [("x", t)], writes=[("x", t)])
                if CUT <= 5:
                    continue
                self.ln_tile(t, None, 1, do_xT=(CUT > 6), do_router=(CUT > 7))

    def load_wq(self, j):
        s = self.sch
        wq = self.arena[:, 4:6, :].rearrange("p s (k n) -> p (s k) n", n=D)
        for st in range(2):
            for h in range(NH):
                s.dma("pool", wq[:, :, h * 128 + st * 64:h * 128 + st * 64 + 64],
                      self.d_wq[j, :, st * 512 + h * 64:st * 512 + h * 64 + 64].rearrange("(k p) n -> p k n", p=128),
                      writes=[("arena", 4), ("arena", 5)])

    def kv_proj(self, es, d_wkv, o_kT, o_v):
        nc, s = self.nc, self.sch
        wk = self.arena[:, 0:2, :].rearrange("p s (k n) -> p (s k) n", n=D)
        wv = self.arena[:, 2:4, :].rearrange("p s (k n) -> p (s k) n", n=D)
        s.dma("pool", wv, d_wkv[:, D:2 * D].rearrange("(k p) n -> p k n", p=128), writes=[("arena", 2), ("arena", 3)])
        for st in range(2):
            for h in range(NH):
                s.dma("pool", wk[:, :, h * 128 + st * 64:h * 128 + st * 64 + 64],
                      d_wkv[:, st * 512 + h * 64:st * 512 + h * 64 + 64].rearrange("(k p) n -> p k n", p=128),
                      writes=[("arena", 0), ("arena", 1)])
        self.load_wq(0)
        vdst = self.kvsrc[:, 512:1024].rearrange("(h c p) (b v) -> c b p h v", h=NH, c=NPIECE, p=128, b=4)
        kst = self.sb("kst", [128, 2, 512], BF16, es)
        vst = self.sb("vst", [128, 2, D], BF16, es)
        vkeys = []
        for t in range(NT_MAIN):
            pb = self.pp[2 + t % 2]
            yk = [("pp", 2 + t % 2, 0), ("pp", 2 + t % 2, 1)]
            for hh in range(2):
                for k in range(KC):
                    s.op("pe", lambda k=k, hh=hh, pb=pb, t=t: nc.tensor.matmul(pb[:, hh * 512:(hh + 1) * 512], self.xT[:, k, t * 128:(t + 1) * 128], wv[:, k, hh * 512:(hh + 1) * 512], start=(k == 0), stop=(k == KC - 1)),
                         reads=[("xT", t), ("arena", 2), ("arena", 3)], writes=[yk[hh]])
            s.op("act", lambda pb=pb, t=t: nc.scalar.copy(vst[:, t % 2, :], pb[:]), reads=yk, writes=[("vst", t % 2)])
            s.dma("sp", vdst[t // 4, t % 4], vst[:, t % 2, :].rearrange("p (h v) -> p h v", v=VD), reads=[("vst", t % 2)], writes=[("kvsrc", "v", t)])
            vkeys.append(("kvsrc", "v", t))
        it = 0
        for h in range(NH):
            kkeys = []
            for ch in range(4):
                tok = slice(ch * 512, ch * 512 + 512)
                pb = self.pp[it % 2]
                pk = ("pp", it % 2, 0)
                for k in range(KC):
                    s.op("pe", lambda k=k, pb=pb, h=h: nc.tensor.matmul(pb[:, 0:512], wk[:, k, h * 128:(h + 1) * 128], self.xT[:, k, tok], start=(k == 0), stop=(k == KC - 1)),
                         reads=[("xT", t) for t in range(ch * 4, ch * 4 + 4)] + [("arena", 0), ("arena", 1)], writes=[pk])
                s.op("act", lambda pb=pb, it=it: nc.scalar.copy(kst[:, it % 2, :], pb[:, 0:512]), reads=[pk], writes=[("kst", it % 2)])
                r0 = (h * 4 + ch) * 128
                s.dma("sp", self.kvsrc[r0:r0 + 128, 0:512], kst[:, it % 2, :], reads=[("kst", it % 2)], writes=[("kvsrc", "k", it)])
                kkeys.append(("kvsrc", "k", it))
                it += 1
            self.kv_exchange_slice(h, vkeys + kkeys)

    def kv_exchange_slice(self, i, keys):
        nc, s = self.nc, self.sch
        s._deps("pool", keys, [])
        RS = KVROWS // KVSPLIT
        nc.gpsimd.collective_compute("AllGather", ALU.bypass, replica_groups=[[0, 1, 2, 3], [4, 5, 6, 7]],
                                     ins=[self.kvsrc[i * RS:(i + 1) * RS, :]],
                                     outs=[self.kvall[i * 4 * RS:(i + 1) * 4 * RS, :]]).then_inc(self.cc_sem)

    USLOT = [4, 8, 12, 16]
    NUNIT = 40

    def attention_layer(self, layer, j):
        nc, s = self.nc, self.sch
        lam_init = 0.8 - 0.6 * math.exp(-0.3 * layer)
        wq = self.arena[:, 4:6, :].rearrange("p s (k n) -> p (s k) n", n=D)
        wo = self.arena[:, 4:6, :].rearrange("p s (k n) -> p (s k) n", n=D)
        if j > 0:
            self.load_wq(j)
        a2 = self.arena[:, 2, :]
        a3 = self.arena[:, 3, :]
        Ebuf = a2[:, 0:3072].rearrange("p (b s n) -> p b s n", b=3, s=2)
        a3f = a3.bitcast(F32)
        gt = a3f[:, 0:1024]
        tmpf = a3f[:, 1024:2048]
        a0 = self.arena[:, 0, :]
        a1f = self.arena[:, 1, :].bitcast(F32)
        KVb = a0.rearrange("p (b n) -> p b n", n=1024)
        self.KVb = KVb
        KTq = KVb[:, :, 0:512]
        Vb = KVb[:, :, 512:1024]
        of = a1f[:, 0:512]
        t2 = a1f[:, 512:1024]
        rs = a1f[:, 1024:1536]
        rz = self.xTf[0:1, :, :].rearrange("p c n -> p (c n)")
        self._attention_body(layer, j, lam_init, wq, wo, Ebuf, gt, tmpf, Vb, KTq, of, t2, rs, rz)

    def _attention_body(self, layer, j, lam_init, wq, wo, Ebuf, gt, tmpf, Vb, KTq, of, t2, rs, rz):
        nc, s = self.nc, self.sch
        l4 = self.lamt[:].rearrange("p (a b d) -> p a b d", a=2, b=2)
        lw = self.lamw
        s.op("dve", lambda: nc.vector.tensor_tensor(self.lamt[:, 0:128].rearrange("p (a d) -> p a d", a=2), l4[:, :, 0, :], l4[:, :, 1, :], ALU.mult),
             reads=["lamt"], writes=["lamt"])
        s.op("dve", lambda: nc.vector.tensor_reduce(lw[:, 0:2], self.lamt[:, 0:128].rearrange("p (a d) -> p a d", a=2), AX.X, ALU.add),
             reads=["lamt"], writes=["lamw"])
        s.op("act", lambda: nc.scalar.activation(out=lw[:, 2:4], in_=lw[:, 0:2], func=AF.Exp), reads=["lamw"], writes=["lamw"])
        s.op("dve", lambda: nc.vector.tensor_tensor(lw[:, 4:5], lw[:, 3:4], lw[:, 2:3], ALU.subtract), reads=["lamw"], writes=["lamw"])
        s.op("dve", lambda: nc.vector.tensor_scalar(lw[:, 5:6], lw[:, 4:5], -lam_init, None, ALU.add), reads=["lamw"], writes=["lamw2"])
        s.op("pe", lambda: nc.tensor.matmul(self.pp[0][:, 0:1], self.ones1f[:], lw[:, 5:6], start=True, stop=True),
             reads=["ones1f", "lamw2"], writes=[("pp", 0, 0)])
        s.op("act", lambda: nc.scalar.copy(self.lamb[:, 0:1], self.pp[0][:, 0:1]), reads=[("pp", 0, 0)], writes=["lamb"])
        s.op("dve", lambda: nc.vector.tensor_scalar(self.sgl[:, 2:3], self.sgl[:, j:j + 1], 1.0 - lam_init, None, ALU.mult), reads=["sgl"], writes=["sgl2"])
        it = 0
        for h in range(NH):
            for sl in range(NPIECE):
                tok = slice(sl * 512, sl * 512 + 512)
                pb = self.pp[it % 2]
                pk = ("pp", it % 2, 0)
                for k in range(KC):
                    s.op("pe", lambda k=k, pb=pb, h=h, tok=tok: nc.tensor.matmul(pb[:, 0:512], wq[:, k, h * 128:(h + 1) * 128], self.xT[:, k, tok], start=(k == 0), stop=(k == KC - 1)),
                         reads=[("xT", t) for t in range(sl * 4, sl * 4 + 4)] + [("arena", 4), ("arena", 5)], writes=[pk])
                s.op("act", lambda pb=pb, h=h, sl=sl: nc.scalar.activation(out=self.qap(h, sl), in_=pb[:, 0:512], func=AF.Copy, scale=HD ** -0.5),
                     reads=[pk], writes=[("qT", h, sl)])
                it += 1
        s.dma("pool", wo, self.d_wo[j].rearrange("(k p) n -> p k n", p=128), writes=[("arena", 4), ("arena", 5)])
        if j == 0:
            self.kv_exchange()
        s.barrier()
        NU = self.NUNIT
        blocks = []
        units = []
        for h in range(NH):
            ubase = 0
            for sl in range(NPIECE):
                for d in reversed(range(self.USLOT[sl])):
                    ui = len(units)
                    units.append((h, ubase + d))
                    for kb in (range(4) if d > 0 else reversed(range(4))):
                        blocks.append(dict(h=h, sl=sl, d=d, kb=kb, unit=ubase + d, ui=ui,
                                           q0=(kb * 128 if d == 0 else 0),
                                           near=(d == 0) or (d == 1 and kb == 3),
                                           first=(d == self.USLOT[sl] - 1 and kb == 0),
                                           last=(d == 0 and kb == 0)))
                ubase += self.USLOT[sl]
        pO, pZ = self.pp[2], self.pp[3]
        kO = [("pp", 2, 0), ("pp", 2, 1)]
        kZ = [("pp", 3, 0), ("pp", 3, 1)]
        st8 = {"sit": 0, "eit": 0, "uload": 0, "nit": 0}
        zS = self.xTf[:].rearrange("p c n -> p (c n)")
        oS = self.lnb[:, 0, :]
        tmpfs = [(tmpf, "tmpf"), (self.lnb[:, 1, :], "tmpf2")]
        pending = []

        def load_units(upto):
            while st8["uload"] <= min(upto, len(units) - 1):
                ui = st8["uload"]
                hh, un = units[ui]
                ub_ = ui % 4
                s.idma(self.KVb[:, ub_, :], self.kvall[:, :], self.idxb[:, hh, un:un + 1], reads=["idx"], writes=[("KTq", ub_), ("Vb", ub_)])
                st8["uload"] += 1

        def emit_S(bk):
            if bk["kb"] == 0:
                load_units(bk["ui"] + 2)
            h, sl, kb, q0 = bk["h"], bk["sl"], bk["kb"], bk["q0"]
            ub_ = bk["ui"] % 4
            nq = 512 - q0
            si = st8["sit"] % 2
            st8["sit"] += 1
            pS = self.pp[si]
            kS = [("pp", si, 0), ("pp", si, 1)]
            bk["pS"], bk["kS"] = pS, kS
            for st in range(2):
                s.op("pe", lambda st=st: nc.tensor.matmul(
                    pS[:, st * 512:st * 512 + nq], KTq[st * 64:(st + 1) * 64, ub_, kb * 128:(kb + 1) * 128],
                    self.qap(h, sl)[st * 64:(st + 1) * 64, q0:512], start=True, stop=True),
                    reads=[("KTq", ub_), ("qT", h, sl)], writes=[kS[st]])

        def emit_head_setup(h):
            s.dma("sp", gt, self.d_gt[h], writes=["gt"])
            s.op("dve", lambda: nc.vector.tensor_scalar(self.fb[:], self.ub[:], self.b31[:, h:h + 1], None, ALU.add),
                 reads=["ub", "b31"], writes=["fb"])

        def emit_exp_pv(bk):
            h, sl, d, kb, q0, unit = bk["h"], bk["sl"], bk["d"], bk["kb"], bk["q0"], bk["unit"]
            ub_ = bk["ui"] % 4
            nq = 512 - q0
            pS, kS = bk["pS"], bk["kS"]
            eb = st8["eit"] % 3
            st8["eit"] += 1
            pS3 = pS[:].rearrange("p (s n) -> p s n", s=2)[:, :, 0:nq]
            E3 = Ebuf[:, eb, :, 0:nq]
            if bk["near"]:
                m0 = q0 + d * 512 - kb * 128 + 384
                tbuf, tkey = tmpfs[st8["nit"] % 2]
                st8["nit"] += 1
                tm3 = tbuf.rearrange("p (s n) -> p s n", s=2)[:, :, 0:nq]
                g3 = gt[:, m0:m0 + nq].unsqueeze(1).to_broadcast([128, 2, nq])
                s.op("dve", lambda: nc.vector.scalar_tensor_tensor(tm3, pS3, self.ub[:, unit:unit + 1], g3, ALU.add, ALU.add),
                     reads=kS + ["gt", "ub"], writes=[tkey])
                s.op("act", lambda: nc.scalar.activation(out=E3, in_=tm3, func=AF.Exp),
                     reads=[tkey], writes=[("E", eb)])
            else:
                s.op("act", lambda: nc.scalar.activation(out=E3, in_=pS3, func=AF.Exp, bias=self.fb[:, unit:unit + 1]),
                     reads=kS + ["fb"], writes=[("E", eb)])
            first, last = bk["first"], bk["last"]
            for st in range(2):
                s.op("pe", lambda st=st: nc.tensor.matmul(
                    pO[:, st * 512 + q0:st * 512 + 512], Vb[:, ub_, kb * 128:(kb + 1) * 128], Ebuf[:, eb, st, 0:nq], start=first, stop=last),
                    reads=[("Vb", ub_), ("E", eb)], writes=[kO[st]])
                s.op("pe", lambda st=st: nc.tensor.matmul(
                    pZ[:, st * 512 + q0:st * 512 + 512], self.onesb[:], Ebuf[:, eb, st, 0:nq], start=first, stop=last),
                    reads=["onesb", ("E", eb)], writes=[kZ[st]])

        def emit_norm(h, sl):
            qcols = sl * 512
            s.op("act", lambda: nc.scalar.copy(zS, pZ[:]), reads=kZ, writes=["xTf"])
            s.op("act", lambda: nc.scalar.copy(oS, pO[:]), reads=kO, writes=["oS"])

            def dveA():
                s.op("dve", lambda: nc.vector.reciprocal(zS, zS), reads=["xTf"], writes=["xTf"])
                s.op("dve", lambda: nc.vector.tensor_tensor(of[:], oS[:, 0:512], zS[:, 0:512], ALU.mult), reads=["oS", "xTf"], writes=["of"])
                s.op("dve", lambda: nc.vector.scalar_tensor_tensor(t2[:], oS[:, 512:1024], self.lamb[:, 0:1], zS[:, 512:1024], ALU.mult, ALU.mult),
                     reads=["oS", "xTf", "lamb"], writes=["t2"])
                s.op("dve", lambda: nc.vector.tensor_tensor(of[:], of[:], t2[:], ALU.add), reads=["of", "t2"], writes=["of"])

            def actSq():
                s.op("act", lambda: nc.scalar.activation(out=t2[:], in_=of[:], func=AF.Square), reads=["of"], writes=["t2"])

            def dveB():
                qi = st8["sit"] % 2
                pQ = self.pp[qi]
                kQ = ("pp", qi, 0)
                s.op("pe", lambda: nc.tensor.matmul(pQ[:, 0:512], self.onesv[:], t2[:], start=True, stop=True), reads=["onesv", "t2"], writes=[kQ])
                s.op("dve", lambda: nc.vector.tensor_scalar(zS[:, 0:512], pQ[:, 0:512], LN_EPS, None, ALU.add), reads=[kQ], writes=["xTf"])
                self.rsqrt_dve(rs[:], zS[:, 0:512], t2[:], ["xTf"], "rs", "t2")
                s.op("dve", lambda: nc.vector.tensor_tensor(of[:], of[:], rs[:], ALU.mult), reads=["of", "rs"], writes=["of"])

            def actOut():
                s.op("act", lambda: nc.scalar.activation(out=self.xT[:, h, qcols:qcols + 512], in_=of[:], func=AF.Copy, scale=self.sgl[:, 2:3]),
                     reads=["of", "sgl2"], writes=[("xT", t) for t in range(sl * 4, sl * 4 + 4)])

            pending.append([1, dveA])
            pending.append([7, actSq])
            pending.append([8, dveB])
            pending.append([15, actOut])

        def run_pending(flush=False):
            for p in pending:
                p[0] -= 1
            pending.sort(key=lambda p: p[0])
            while pending and (flush or pending[0][0] <= 0):
                pending.pop(0)[1]()

        emit_S(blocks[0])
        for i, bk in enumerate(blocks):
            if i + 1 < len(blocks):
                emit_S(blocks[i + 1])
            if bk["sl"] == 0 and bk["first"]:
                emit_head_setup(bk["h"])
            emit_exp_pv(bk)
            run_pending()
            if bk["last"]:
                emit_norm(bk["h"], bk["sl"])
        run_pending(flush=True)
        self.load_lnb(layer, 0)

        def oproj(t):
            py = self.pp[t % 2]
            yk = [("pp", t % 2, 0), ("pp", t % 2, 1)]
            for hh in range(2):
                for h in range(NH):
                    s.op("pe", lambda h=h, hh=hh: nc.tensor.matmul(py[:, hh * 512:(hh + 1) * 512], self.xT[:, h, t * 128:(t + 1) * 128], wo[:, h, hh * 512:(hh + 1) * 512], start=(h == 0), stop=(h == NH - 1)),
                         reads=[("xT", t), ("arena", 4), ("arena", 5)], writes=[yk[hh]])

        oproj(0)
        for t in range(NT_MAIN):
            if t + 1 < NT_MAIN:
                oproj(t + 1)
            py = self.pp[t % 2]
            yk = [("pp", t % 2, 0), ("pp", t % 2, 1)]
            s.op("dve", lambda: nc.vector.scalar_tensor_tensor(self.x[:, t, :], self.x[:, t, :], ALPHA, py[:], ALU.mult, ALU.add),
                 reads=yk + [("x", t)], writes=[("x", t)])
            self.ln_tile(t, None, 1, do_xT=True, do_router=True)


def _core_tokens(c):
    b, r = divmod(c, 4)
    pos = piece_positions(r)
    return b, pos


def t5_bucket_np(n):
    n = np.maximum(n, 0)
    nf = np.maximum(n, 1).astype(np.float32)
    large = 16 + (np.log(nf / np.float32(16)) / np.float32(math.log(128 / 16)) * np.float32(16)).astype(np.int32)
    large = np.minimum(large, 31)
    return np.where(n < 16, n, large)


def prep_F(inp):
    x = np.asarray(inp["x"], dtype=np.float32)
    rel_bias = np.asarray(inp["rel_bias"], np.float32)
    ki = np.arange(128)[:, None]
    m = np.arange(1024)[None, :]
    rel = m - ki - 384
    bidx = t5_bucket_np(rel)
    gtab = np.empty((NH, 128, 1024), np.float32)
    for h in range(NH):
        g = rel_bias[bidx, h]
        gtab[h] = np.where(rel >= 0, g, np.float32(NEG))
    shared = {
        "ident": np.eye(128, dtype=np.float32),
        "rw": np.ascontiguousarray(np.asarray(inp["router_w"], np.float32).reshape(KC, 128, NE).transpose(1, 0, 2).reshape(128, KC * NE)),
        "rbias": np.ascontiguousarray(np.broadcast_to(np.asarray(inp["router_bias"], np.float32)[None, :], (128, NE))),
        "moe_w_gate": np.asarray(inp["moe_w_gate"], np.float32),
        "moe_w_up": np.asarray(inp["moe_w_up"], np.float32),
        "moe_w_down": np.asarray(inp["moe_w_down"], np.float32),
        "a_w_pw1": np.asarray(inp["a_w_pw1"], np.float32),
        "a_w_pw2": np.asarray(inp["a_w_pw2"], np.float32),
        "a_b_pw2": np.asarray(inp["a_b_pw2"], np.float32),
        "w_kv": np.asarray(inp["w_kv"], np.float32),
        "gtab": gtab,
        "b31r": np.ascontiguousarray(np.broadcast_to(rel_bias[31][None, :], (128, NH))),
        "b_w_q": np.asarray(inp["b_w_q"], np.float32),
        "b_w_o": np.asarray(inp["b_w_o"], np.float32),
        "b_lambda": np.asarray(inp["b_lambda"], np.float32).reshape(2, 1, 256),
        "sublng": np.ascontiguousarray(np.asarray(inp["b_subln_g"], np.float32).T),
    }
    lnb = np.empty((DEPTH, 4, 128, D), np.float32)
    for l in range(DEPTH):
        for i, k in enumerate(("ln_mix_g", "ln_mix_b", "ln_ffn_g", "ln_ffn_b")):
            lnb[l, i] = np.asarray(inp[k], np.float32)[l][None, :]
    shared["lnb"] = lnb
    cfm = np.empty((N_A, 128, 288), np.float32)
    for l in range(N_A):
        cfm[l, :, 0:16] = np.asarray(inp["a_b_pw1"], np.float32)[l].reshape(16, 128).T
        wdw = np.asarray(inp["a_w_dw"], np.float32)[l]
        cfm[l, :, 16:264] = wdw.reshape(CONVW, KC, 128).transpose(2, 1, 0).reshape(128, KC * CONVW)
        cfm[l, :, 264:272] = np.asarray(inp["a_b_dw"], np.float32)[l].reshape(KC, 128).T
        cfm[l, :, 272:280] = np.asarray(inp["a_ln_g"], np.float32)[l].reshape(KC, 128).T
        cfm[l, :, 280:288] = np.asarray(inp["a_ln_b"], np.float32)[l].reshape(KC, 128).T
    shared["cfm"] = cfm
    owner = {}
    for r in range(4):
        for p, g in enumerate(piece_positions(r)):
            owner[g] = (r, p)
    NU = Prog.NUNIT
    parr = np.arange(128, dtype=np.int64)
    maps = []
    for c in range(NCORES):
        b, pos = _core_tokens(c)
        xin = np.zeros(((NT_MAIN + NT_HALO) * 128, D), np.float32)
        hvv = np.zeros((128, NPIECE), np.float32)
        for p, ps_ in enumerate(pos):
            xin[p * 512:(p + 1) * 512] = x[b, ps_ * 512:(ps_ + 1) * 512]
            if ps_ > 0:
                xin[(16 + p) * 128:(17 + p) * 128] = x[b, ps_ * 512 - 128:ps_ * 512]
                hvv[:, p] = 1.0
        ub = np.zeros((128, NU), np.float32)
        kvidx = np.zeros((NH, 128, NU), np.int32)
        u = 0
        for sl in range(NPIECE):
            for d in range(Prog.USLOT[sl]):
                g = pos[sl] - d
                if g < 0:
                    ub[:, u] = NEG
                    g = 0
                r2, p2 = owner[g]
                RS = KVROWS // KVSPLIT
                for h in range(NH):
                    row = (h * 4 + p2) * 128
                    kvidx[h, :, u] = (row // RS) * 4 * RS + r2 * RS + row % RS + parr
                u += 1
        mm = dict(shared)
        mm["xin"] = xin
        mm["halo_valid"] = hvv
        mm["ubias"] = ub
        mm["kvidx"] = kvidx
        maps.append(mm)
    return maps


_PROGS = {}


def get_prog():
    if "F" not in _PROGS:
        _PROGS["F"] = Prog("F").build()
    return _PROGS["F"]


def kernel(**inputs):
    maps = prep_F(inputs)
    nc = get_prog()
    res = run_bass_kernel_spmd(nc, maps, core_ids=list(range(NCORES))).results
    out = np.empty((BATCH, SEQ, D), np.float32)
    for c in range(NCORES):
        b, pos = _core_tokens(c)
        xo = np.asarray(res[c]["x_out"], np.float32)
        for p, g in enumerate(pos):
            out[b, g * 512:(g + 1) * 512] = xo[p * 512:(p + 1) * 512]
    return out
```

```python
import math
import os
from contextlib import ExitStack
CUT = int(os.environ.get("KCUT", "99"))
RCUT = int(os.environ.get("RCUT", "99"))

import numpy as np
import ml_dtypes

import concourse.bass as bass
import concourse.mybir as mybir
from concourse.bass_utils import run_bass_kernel_spmd

F32 = mybir.dt.float32
BF16 = mybir.dt.bfloat16
AF = mybir.ActivationFunctionType
ALU = mybir.AluOpType
AX = mybir.AxisListType

NCORES = 8
D = 1024
SEQ = 8192
BATCH = 2
DEPTH = 4
N_A = 2
KC = 8
NPIECE = 4
NT_MAIN = 16
NT_HALO = 4
CONVW = 31
NE = 16
DEXP = 512
NH = 8
HD = 64
VD = 128
ALPHA = (2.0 * DEPTH) ** 0.25
LN_EPS = 1e-5
NEG = -30000.0
KVROWS = 4096
KVSPLIT = 8


def piece_positions(r):
    return [r, 7 - r, 8 + r, 15 - r]


class Sched:
    LIMIT = 60000

    def __init__(self, nc, es, ndma=48, strict=("act", "dve", "pool")):
        self.nc = nc
        self.es = es
        self.engs = {"pe": nc.tensor, "act": nc.scalar, "dve": nc.vector,
                     "pool": nc.gpsimd, "sp": nc.sync}
        self.semh = {}
        self.nep = {k: 0 for k in self.engs}
        self.cnt = {k: 0 for k in self.engs}
        for k in self.engs:
            self._new_epoch(k)
        self.waited = {k: {} for k in self.engs}
        self.strict = set(strict)
        self.ndma = ndma
        for i in range(ndma):
            self.semh[("d", i)] = es.enter_context(nc.semaphore(f"dq{i}"))
        self.dval = [0] * ndma
        self.dpool = {"sp": list(range(0, ndma // 2)), "pool": list(range(ndma // 2, ndma))}
        self.dnext = {"sp": 0, "pool": 0}
        self.bufs = {}
        self.nwaits = 0
        self.nops = {k: 0 for k in self.engs}

    def _new_epoch(self, k):
        ep = self.nep[k]
        self.semh[("e", k, ep)] = self.es.enter_context(self.nc.semaphore(f"s_{k}{ep}"))
        self.nep[k] = ep + 1
        self.cnt[k] = 0

    def _buf(self, key):
        b = self.bufs.get(key)
        if b is None:
            b = {"w": {}, "r": {}}
            self.bufs[key] = b
        return b

    def _wait(self, eng, tok):
        semkey, val = tok
        if semkey[0] == "e":
            if semkey[1] == eng and eng not in self.strict:
                return
            w = self.waited[eng]
            for ep in range(semkey[2] + 1, self.nep[semkey[1]]):
                if ("e", semkey[1], ep) in w:
                    return
        else:
            w = self.waited[eng]
        if w.get(semkey, 0) >= val:
            return
        self.engs[eng].wait_ge(self.semh[semkey], val)
        w[semkey] = val
        self.nwaits += 1

    def _deps(self, eng, reads, writes):
        toks = {}
        for k in reads:
            b = self._buf(k)
            for src, t in b["w"].items():
                toks[(src, t)] = t
        for k in writes:
            b = self._buf(k)
            for src, t in b["w"].items():
                toks[(src, t)] = t
            for src, t in b["r"].items():
                toks[(src, t)] = t
        for t in toks.values():
            self._wait(eng, t)

    def op(self, eng, fn, reads=(), writes=()):
        self._deps(eng, reads, writes)
        ins = fn()
        self.cnt[eng] += 1
        self.nops[eng] += 1
        ep = self.nep[eng] - 1
        semkey = ("e", eng, ep)
        ins.then_inc(self.semh[semkey], 1)
        tok = (semkey, self.cnt[eng])
        if self.cnt[eng] >= self.LIMIT:
            self._new_epoch(eng)
        src = ("e", eng)
        for k in writes:
            self._buf(k)["w"][src] = tok
        for k in reads:
            self._buf(k)["r"][src] = tok
        return tok

    def dma(self, q, out, in_, reads=(), writes=()):
        self._deps(q, reads, writes)
        pl = self.dpool[q]
        i = pl[self.dnext[q]]
        self.dnext[q] = (self.dnext[q] + 1) % len(pl)
        semkey = ("d", i)
        if self.dval[i] > 0:
            self._wait(q, (semkey, self.dval[i]))
        assert self.dval[i] + 16 < 65000
        ins = self.engs[q].dma_start(out=out, in_=in_)
        self.dval[i] += 16
        ins.then_inc(self.semh[semkey], 16)
        tok = (semkey, self.dval[i])
        for k in writes:
            self._buf(k)["w"][semkey] = tok
        for k in reads:
            self._buf(k)["r"][semkey] = tok
        return tok

    def idma(self, out, in_, idx_ap, reads=(), writes=()):
        q = "pool"
        if os.environ.get("NOIDMA"):
            return self.dma(q, out, in_[0:128, :], reads=reads, writes=writes)
        self._deps(q, reads, writes)
        pl = self.dpool[q]
        i = pl[self.dnext[q]]
        self.dnext[q] = (self.dnext[q] + 1) % len(pl)
        semkey = ("d", i)
        if self.dval[i] > 0:
            self._wait(q, (semkey, self.dval[i]))
        assert self.dval[i] + 16 < 65000
        ins = self.nc.gpsimd.indirect_dma_start(out=out, out_offset=None, in_=in_,
                                                in_offset=bass.IndirectOffsetOnAxis(ap=idx_ap, axis=0))
        self.dval[i] += 16
        ins.then_inc(self.semh[semkey], 16)
        tok = (semkey, self.dval[i])
        for k in writes:
            self._buf(k)["w"][semkey] = tok
        for k in reads:
            self._buf(k)["r"][semkey] = tok
        return tok

    def barrier(self):
        toks = []
        for k in self.engs:
            if self.cnt[k] > 0:
                toks.append((("e", k, self.nep[k] - 1), self.cnt[k]))
            elif self.nep[k] > 1:
                toks.append((("e", k, self.nep[k] - 2), self.LIMIT))
        for i in range(self.ndma):
            if self.dval[i] > 0:
                toks.append((("d", i), self.dval[i]))
        for k in self.engs:
            st = self.strict
            self.strict = set(self.engs)
            for t in toks:
                if t[0][0] == "e" and t[0][1] == k:
                    continue
                self._wait(k, t)
            self.strict = st

    def finish(self):
        for i in range(self.ndma):
            if self.dval[i] > 0:
                self._wait("sp", (("d", i), self.dval[i]))
        for k in self.engs:
            if k == "sp":
                continue
            if self.cnt[k] > 0:
                self._wait("sp", (("e", k, self.nep[k] - 1), self.cnt[k]))


class Prog:
    def __init__(self, mode, stage=99):
        self.stage = stage
        self.mode = mode
        self.nc = bass.Bass("TRN2", target_bir_lowering=False)
        self.es = ExitStack()

    def dram_in(self, name, shape, dt=F32):
        return self.nc.dram_tensor(name, list(shape), dt, kind="ExternalInput").ap()

    def dram_out(self, name, shape, dt=F32):
        return self.nc.dram_tensor(name, list(shape), dt, kind="ExternalOutput").ap()

    def sb(self, name, shape, dt, es=None):
        return (es or self.es).enter_context(self.nc.sbuf_tensor("sb_" + name, list(shape), dt))

    def ps(self, name, shape, dt=F32, es=None):
        return (es or self.es).enter_context(self.nc.psum_tensor("ps_" + name, list(shape), dt))

    def build(self):
        with self.es:
            self.sch = Sched(self.nc, self.es)
            self._build_F()
            self.sch.finish()
        return self.nc

    def _common_alloc(self, ntiles):
        nc = self.nc
        self.ntiles = ntiles
        self.x = self.sb("x", [128, ntiles, D], F32)
        self.xT = self.sb("xT", [128, KC, ntiles * 128], BF16)
        self.arena = self.sb("arena", [128, 6, 4096], BF16)
        self.lnb = self.sb("lnb", [128, 2, D], F32)
        self.gates = self.sb("gates", [128, ntiles, NE], F32)
        self.ident = self.sb("ident", [128, 128], F32)
        self.rw = self.sb("rw", [128, KC, NE], F32)
        self.rbias = self.sb("rbias", [128, NE], F32)
        self.consts = self.sb("consts", [128, 8], F32)
        self.stat = self.sb("stat", [128, 2, 6], F32)
        self.mv = self.sb("mv", [128, 8], F32)
        self.xTf = self.sb("xTf", [128, KC, 128], F32)
        self.rt = self.sb("rt", [128, 8, NE], F32)
        self.pp = [self.ps(f"pp{i}", [128, 1024], F32) for i in range(4)]
        self.d_ident = self.dram_in("ident", [128, 128])
        self.d_rw = self.dram_in("rw", [128, KC * NE])
        self.d_rbias = self.dram_in("rbias", [128, NE])
        self.d_lnb = self.dram_in("lnb", [DEPTH, 4, 128, D])
        nl = getattr(self, "n_moe_layers", DEPTH)
        ne_decl = NE if nl > 0 else 1
        nl = max(nl, 1)
        self.d_wg = self.dram_in("moe_w_gate", [nl, ne_decl, D, DEXP])
        self.d_wu = self.dram_in("moe_w_up", [nl, ne_decl, D, DEXP])
        self.d_wd = self.dram_in("moe_w_down", [nl, ne_decl, DEXP, D])
        s = self.sch
        s.dma("sp", self.ident[:], self.d_ident[:, :], writes=["ident"])
        s.dma("sp", self.rw[:].rearrange("p k e -> p (k e)"), self.d_rw[:, :], writes=["rw"])
        s.dma("sp", self.rbias[:], self.d_rbias[:, :], writes=["rbias"])
        s.op("pool", lambda: nc.gpsimd.memset(self.consts[:, 0:1], -0.5), writes=["consts"])
        s.op("pool", lambda: nc.gpsimd.memset(self.consts[:, 1:2], LN_EPS), writes=["consts"])
        s.op("pool", lambda: nc.gpsimd.memset(self.consts[:, 2:3], LN_EPS / (ALPHA * ALPHA)), writes=["consts"])
        self.wslot = 0

    def slot_gu(self, i):
        return self.arena[:, i, :].rearrange("p (k n) -> p k n", n=DEXP)

    def slot_d(self, i):
        return self.arena[:, i, :].rearrange("p (k n) -> p k n", n=D)

    def rsqrt_dve(self, out, in_, tmp, rkeys, okey, tkey):
        nc, s = self.nc, self.sch
        I32 = mybir.dt.int32
        s.op("dve", lambda: nc.vector.tensor_scalar(out.bitcast(I32), in_.bitcast(I32), 1, None, ALU.arith_shift_right),
             reads=rkeys, writes=[okey])
        s.op("dve", lambda: nc.vector.tensor_scalar(out.bitcast(I32), out.bitcast(I32), -1.0, 1597463007.0, ALU.mult, ALU.add),
             reads=[okey], writes=[okey])
        for _ in range(3):
            s.op("dve", lambda: nc.vector.tensor_tensor(tmp, out, out, ALU.mult), reads=[okey], writes=[tkey])
            s.op("dve", lambda: nc.vector.tensor_tensor(tmp, tmp, in_, ALU.mult), reads=[tkey] + rkeys, writes=[tkey])
            s.op("dve", lambda: nc.vector.tensor_scalar(tmp, tmp, -0.5, 1.5, ALU.mult, ALU.add), reads=[tkey], writes=[tkey])
            s.op("dve", lambda: nc.vector.tensor_tensor(out, out, tmp, ALU.mult), reads=[okey, tkey], writes=[okey])

    def ln_tile(self, t, src_key, eps_col, do_xT=True, do_router=True, last=False):
        nc, s = self.nc, self.sch
        xt = self.x[:, t, :]
        xk = ("x", t)
        for hh in range(2):
            s.op("dve", lambda hh=hh: nc.vector.bn_stats(self.stat[:, hh, :], self.x[:, t, hh * 512:(hh + 1) * 512]),
                 reads=[xk], writes=["stat"])
        s.op("dve", lambda: nc.vector.bn_aggr(self.mv[:, 0:2], self.stat[:].rearrange("p a b -> p (a b)")),
             reads=["stat"], writes=["mv"])
        s.op("dve", lambda: nc.vector.tensor_scalar(self.mv[:, 2:3], self.mv[:, 1:2], self.consts[:, eps_col:eps_col + 1], None, ALU.add),
             reads=["mv", "consts"], writes=["mv2"])
        self.rsqrt_dve(self.mv[:, 3:4], self.mv[:, 2:3], self.mv[:, 4:5], ["mv2"], "mv3", "mv4")
        s.op("dve", lambda: nc.vector.tensor_scalar(xt, xt, self.mv[:, 0:1], self.mv[:, 3:4], ALU.subtract, ALU.mult),
             reads=[xk, "mv", "mv3"], writes=[xk])
        s.op("dve", lambda: nc.vector.tensor_tensor(xt, xt, self.lnb[:, 0, :], ALU.mult),
             reads=[xk, "lnb"], writes=[xk])
        s.op("dve", lambda: nc.vector.tensor_tensor(xt, xt, self.lnb[:, 1, :], ALU.add),
             reads=[xk, "lnb"], writes=[xk])
        if not do_xT:
            return
        pt = self.pp[3]
        for c in range(KC):
            s.op("pe", lambda c=c: nc.tensor.transpose(pt[:, c * 128:(c + 1) * 128], self.x[:, t, c * 128:(c + 1) * 128], self.ident[:]),
                 reads=[xk, "ident"], writes=[("pp", 3, c // 4)])
        xTk = ("xT", t)
        if not do_router:
            s.op("act", lambda: nc.scalar.copy(self.xT[:, :, t * 128:(t + 1) * 128], pt[:].rearrange("p (c n) -> p c n", n=128)),
                 reads=[("pp", 3, 0), ("pp", 3, 1)], writes=[xTk])
            return
        s.op("act", lambda: nc.scalar.copy(self.xTf[:], pt[:].rearrange("p (c n) -> p c n", n=128)),
             reads=[("pp", 3, 0), ("pp", 3, 1)], writes=["xTf"])
        s.op("dve", lambda: nc.vector.tensor_copy(self.xT[:, :, t * 128:(t + 1) * 128], self.xTf[:]),
             reads=["xTf"], writes=[xTk])
        if RCUT <= 1:
            return
        pr = self.pp[2]
        for c in range(KC):
            s.op("pe", lambda c=c: nc.tensor.matmul(pr[:, 0:NE], self.xTf[:, c, :], self.rw[:, c, :], start=(c == 0), stop=(c == KC - 1)),
                 reads=["xTf", "rw"], writes=[("pp", 2, 0)])
        if RCUT <= 2:
            return
        self.router_gates(t, pr[:, 0:NE], ("pp", 2, 0))

    def router_gates(self, t, logits_ps, pkey):
        nc, s = self.nc, self.sch
        rt = self.rt
        aff, sel, tmp, eq, msk = rt[:, 0, :], rt[:, 1, :], rt[:, 2, :], rt[:, 3, :], rt[:, 4, :]
        m1, m2, gs, gsel = rt[:, 5, 0:4], rt[:, 5, 4:8], rt[:, 5, 8:12], rt[:, 5, 12:16]
        gmax, wsum = rt[:, 6, 0:1], rt[:, 6, 1:2]
        R = ["rt"]
        g3 = lambda a: a.rearrange("p (g e) -> p g e", e=4)
        s.op("act", lambda: nc.scalar.activation(out=aff, in_=logits_ps, func=AF.Sigmoid), reads=[pkey], writes=R)
        s.op("dve", lambda: nc.vector.tensor_tensor(sel, aff, self.rbias[:], ALU.add), reads=R + ["rbias"], writes=R)
        s.op("dve", lambda: nc.vector.tensor_reduce(m1, g3(sel), AX.X, ALU.max), reads=R, writes=R)
        s.op("dve", lambda: nc.vector.tensor_tensor(g3(eq), g3(sel), self._b3(m1), ALU.is_equal), reads=R, writes=R)
        s.op("dve", lambda: nc.vector.scalar_tensor_tensor(tmp, eq, -1.0e9, sel, ALU.mult, ALU.add), reads=R, writes=R)
        s.op("dve", lambda: nc.vector.tensor_reduce(m2, g3(tmp), AX.X, ALU.max), reads=R, writes=R)
        s.op("dve", lambda: nc.vector.tensor_tensor(gs, m1, m2, ALU.add), reads=R, writes=R)
        s.op("dve", lambda: nc.vector.tensor_reduce(gmax, gs, AX.X, ALU.max), reads=R, writes=R)
        s.op("dve", lambda: nc.vector.tensor_scalar(gsel, gs, gmax, None, ALU.is_equal), reads=R, writes=R)
        s.op("dve", lambda: nc.vector.tensor_tensor(g3(msk), g3(sel), self._b3(m2), ALU.is_ge), reads=R, writes=R)
        s.op("dve", lambda: nc.vector.tensor_tensor(g3(msk), g3(msk), self._b3(gsel), ALU.mult), reads=R, writes=R)
        s.op("dve", lambda: nc.vector.tensor_tensor(tmp, aff, msk, ALU.mult), reads=R, writes=R)
        s.op("dve", lambda: nc.vector.tensor_reduce(wsum, tmp, AX.X, ALU.add), reads=R, writes=R)
        s.op("dve", lambda: nc.vector.reciprocal(wsum, wsum), reads=R, writes=R)
        s.op("dve", lambda: nc.vector.tensor_scalar(self.gates[:, t, :], tmp, wsum, 1.0 / ALPHA, ALU.mult, ALU.mult),
             reads=R, writes=[("gates", t)])

    def _b3(self, a):
        return a.unsqueeze(2).to_broadcast([128, 4, 4])

    def moe(self, layer, chunks, es, tail=None):
        nc, s = self.nc, self.sch
        sg = self.sb(f"moe_s{layer}", [128, 2, 512], BF16, es)
        hT = self.sb(f"moe_h{layer}", [128, 2, 4, 512], BF16, es)
        it = 0
        for e in range(NE):
            sl = []
            for j, (dw, nk) in enumerate(((self.d_wg, KC), (self.d_wu, KC), (self.d_wd, 4))):
                i = self.wslot
                self.wslot = (self.wslot + 1) % 6
                dst = self.slot_gu(i) if j < 2 else self.slot_d(i)
                s.dma("pool", dst, dw[layer, e].rearrange("(k p) n -> p k n", p=128), writes=[("arena", i)])
                sl.append(i)
            wg, wu, wd = self.slot_gu(sl[0]), self.slot_gu(sl[1]), self.slot_d(sl[2])
            for ch in chunks:
                t0 = ch[0]
                tok = slice(t0 * 128, t0 * 128 + 512)
                hb = it % 2
                for j in range(4):
                    pb = self.pp[(it * 4 + j) % 2]
                    pg, pu = pb[:, 0:512], pb[:, 512:1024]
                    kg, ku = ("pp", (it * 4 + j) % 2, 0), ("pp", (it * 4 + j) % 2, 1)
                    xk = [("xT", t) for t in ch]
                    for k in range(KC):
                        s.op("pe", lambda k=k, j=j, pg=pg: nc.tensor.matmul(pg, wg[:, k, j * 128:(j + 1) * 128], self.xT[:, k, tok], start=(k == 0), stop=(k == KC - 1)),
                             reads=xk + [("arena", sl[0])], writes=[kg])
                    for k in range(KC):
                        s.op("pe", lambda k=k, j=j, pu=pu: nc.tensor.matmul(pu, wu[:, k, j * 128:(j + 1) * 128], self.xT[:, k, tok], start=(k == 0), stop=(k == KC - 1)),
                             reads=xk + [("arena", sl[1])], writes=[ku])
                    sb_ = (it * 4 + j) % 2
                    s.op("act", lambda pg=pg, sb_=sb_: nc.scalar.activation(out=sg[:, sb_, :], in_=pg, func=AF.Silu),
                         reads=[kg], writes=[("moe_s", sb_)])
                    s.op("dve", lambda pu=pu, sb_=sb_, j=j, hb=hb: nc.vector.tensor_tensor(hT[:, hb, j, :], pu, sg[:, sb_, :], ALU.mult),
                         reads=[ku, ("moe_s", sb_)], writes=[("moe_h", hb)])
                for tt, t in enumerate(ch):
                    py = self.pp[2 + (it * 4 + tt) % 2]
                    yk = [("pp", 2 + (it * 4 + tt) % 2, 0), ("pp", 2 + (it * 4 + tt) % 2, 1)]
                    for hh in range(2):
                        for j in range(4):
                            s.op("pe", lambda hh=hh, j=j, tt=tt, py=py, hb=hb: nc.tensor.matmul(py[:, hh * 512:(hh + 1) * 512], hT[:, hb, j, tt * 128:(tt + 1) * 128], wd[:, j, hh * 512:(hh + 1) * 512], start=(j == 0), stop=(j == 3)),
                                 reads=[("moe_h", hb), ("arena", sl[2])], writes=[yk[hh]])
                    s.op("dve", lambda t=t, py=py: nc.vector.scalar_tensor_tensor(self.x[:, t, :], py[:], self.gates[:, t, e:e + 1], self.x[:, t, :], ALU.mult, ALU.add),
                         reads=yk + [("gates", t), ("x", t)], writes=[("x", t)])
                it += 1
                if tail is not None and e == NE - 1:
                    tail(ch)

    def load_lnb(self, layer, which):
        s = self.sch
        s.dma("sp", self.lnb[:, 0, :], self.d_lnb[layer, 2 * which], writes=["lnb", "oS"])
        s.dma("sp", self.lnb[:, 1, :], self.d_lnb[layer, 2 * which + 1], writes=["lnb", "tmpf2"])

    def _build_F(self):
        nc = self.nc
        self.n_moe_layers = DEPTH
        self._common_alloc(NT_MAIN + NT_HALO)
        s = self.sch
        d_x = self.dram_in("xin", [(NT_MAIN + NT_HALO) * 128, D])
        d_hv = self.dram_in("halo_valid", [128, NPIECE])
        d_pw1 = self.dram_in("a_w_pw1", [N_A, D, 2 * D])
        d_pw2 = self.dram_in("a_w_pw2", [N_A, D, D])
        d_cfm = self.dram_in("cfm", [N_A, 128, 288])
        d_bpw2 = self.dram_in("a_b_pw2", [N_A, D])
        d_wkv = self.dram_in("w_kv", [D, 2 * D])
        d_ub = self.dram_in("ubias", [128, self.NUNIT])
        self.d_gt = self.dram_in("gtab", [NH, 128, 1024])
        d_b31 = self.dram_in("b31r", [128, NH])
        self.d_wq = self.dram_in("b_w_q", [2, D, D])
        self.d_wo = self.dram_in("b_w_o", [2, D, D])
        d_lam = self.dram_in("b_lambda", [2, 1, 256])
        d_sg = self.dram_in("sublng", [128, 2])
        self.d_idx = self.dram_in("kvidx", [NH, 128, self.NUNIT], mybir.dt.int32)
        o_x = self.dram_out("x_out", [NT_MAIN * 128, D])
        self.kvsrc = nc.dram_tensor("kvsrc", [KVROWS, 1024], BF16, kind="Internal").ap()
        self.kvall = nc.dram_tensor("kvall", [4 * KVROWS, 1024], BF16, kind="Internal").ap()
        self.cc_sem = self.es.enter_context(nc.semaphore("cc_sem"))
        self.w1s = nc.dram_tensor("w1s", [N_A, KC, 128, KC * 256], BF16, kind="Internal").ap()
        for layer in range(N_A):
            for c in range(KC):
                dst = self.w1s[layer, c].rearrange("p (k n) -> p k n", n=256)
                s.dma("pool", dst[:, :, 0:128], d_pw1[layer, :, c * 128:(c + 1) * 128].rearrange("(k p) n -> p k n", p=128), writes=[("w1s", layer, c)])
                s.dma("pool", dst[:, :, 128:256], d_pw1[layer, :, D + c * 128:D + (c + 1) * 128].rearrange("(k p) n -> p k n", p=128), writes=[("w1s", layer, c)])

        with ExitStack() as esA:
            hv = self.sb("hv", [128, NPIECE], F32, esA)
            cfm = self.sb("cfm", [128, 288], F32, esA)
            ghist = self.sb("ghist", [128, NPIECE, KC, 30], F32, esA)
            browf = self.sb("browf", [1, D], F32, esA)
            ones1 = self.sb("ones1", [1, 128], F32, esA)
            onesm = self.sb("onesm", [128, 128], F32, esA)
            s.dma("sp", hv[:], d_hv[:, :], writes=["hv"])
            s.op("pool", lambda: nc.gpsimd.memset(ones1[:], 1.0), writes=["ones1"])
            s.op("pool", lambda: nc.gpsimd.memset(onesm[:], 1.0 / D), writes=["onesm"])
            for t in range(self.ntiles):
                s.dma("sp", self.x[:, t, :], d_x[t * 128:(t + 1) * 128, :], writes=[("x", t)])
            for t in range(self.ntiles):
                self.transpose_only(t)
            for layer in range(N_A):
                with ExitStack() as es:
                    self.conformer_layer(layer, es, d_pw1, d_pw2, d_cfm, d_bpw2, hv, cfm, ghist, None, browf, ones1, onesm)
                s.barrier()
                with ExitStack() as es:
                    nch = 5 if layer == 0 else 4
                    chunks = [[4 * c + i for i in range(4)] for c in range(nch)]
                    self.load_lnb(layer, 1)
                    self.moe(layer, chunks, es, tail=lambda ch: [self.ln_tile(t, None, 2, do_xT=True, do_router=False) for t in ch])
                s.barrier()
            with ExitStack() as es:
                self.kv_proj(es, d_wkv, None, None)
            s.barrier()

        with ExitStack() as esB:
            self.qT0 = self.sb("qT0", [128, NH, 512], BF16, esB)
            self.ub = self.sb("ub", [128, self.NUNIT], F32, esB)
            self.fb = self.sb("fb", [128, self.NUNIT], F32, esB)
            self.b31 = self.sb("b31", [128, NH], F32, esB)
            self.lamt = self.sb("lamt", [1, 256], F32, esB)
            self.lamw = self.sb("lamw", [1, 8], F32, esB)
            self.sgl = self.sb("sgl", [128, 4], F32, esB)
            self.ones1f = self.sb("ones1f", [1, 128], F32, esB)
            self.onesv = self.sb("onesv", [128, 128], F32, esB)
            self.onesb = self.sb("onesb", [128, 128], BF16, esB)
            self.lamb = self.sb("lamb", [128, 1], F32, esB)
            self.idxb = self.sb("idxb", [128, NH, self.NUNIT], mybir.dt.int32, esB)
            s.dma("sp", self.idxb[:], self.d_idx.rearrange("h p u -> p h u"), writes=["idx"])
            s.dma("sp", self.ub[:], d_ub[:, :], writes=["ub"])
            s.dma("sp", self.b31[:], d_b31[:, :], writes=["b31"])
            s.dma("sp", self.sgl[:, 0:2], d_sg[:, :], writes=["sgl"])
            s.op("pool", lambda: nc.gpsimd.memset(self.ones1f[:], 1.0), writes=["ones1f"])
            s.op("pool", lambda: nc.gpsimd.memset(self.onesv[:], 1.0 / VD), writes=["onesv"])
            s.op("pool", lambda: nc.gpsimd.memset(self.onesb[:], 1.0), writes=["onesb"])
            for j in range(2):
                layer = N_A + j
                s.dma("sp", self.lamt[:], d_lam[j], writes=["lamt"])
                self.attention_layer(layer, j)
                s.barrier()
                with ExitStack() as es:
                    chunks = [[4 * c + i for i in range(4)] for c in range(4)]
                    self.load_lnb(layer, 1)
                    self.moe(layer, chunks, es, tail=lambda ch, j=j: [self.ln_tile(t, None, 2, do_xT=(j == 0), do_router=False) for t in ch])
                s.barrier()
            for t in range(NT_MAIN):
                s.dma("sp", o_x[t * 128:(t + 1) * 128, :], self.x[:, t, :], reads=[("x", t)])

    def kv_exchange(self):
        self.nc.gpsimd.wait_ge(self.cc_sem, KVSPLIT)

    def qap(self, h, sl):
        if sl == 0:
            return self.qT0[:, h, :]
        if sl == 3:
            return self.xT[:, h, NT_MAIN * 128:NT_MAIN * 128 + 512]
        t = NT_MAIN + 2 * (sl - 1) + h // 4
        return self.x[:, t, :].bitcast(BF16)[:, (h % 4) * 512:(h % 4 + 1) * 512]

    def transpose_only(self, t):
        nc, s = self.nc, self.sch
        pt = self.pp[3]
        xk = ("x", t)
        for c in range(KC):
            s.op("pe", lambda c=c: nc.tensor.transpose(pt[:, c * 128:(c + 1) * 128], self.x[:, t, c * 128:(c + 1) * 128], self.ident[:]),
                 reads=[xk, "ident"], writes=[("pp", 3, c // 4)])
        s.op("act", lambda: nc.scalar.copy(self.xT[:, :, t * 128:(t + 1) * 128], pt[:].rearrange("p (c n) -> p c n", n=128)),
             reads=[("pp", 3, 0), ("pp", 3, 1)], writes=[("xT", t)])

    def conformer_layer(self, layer, es, d_pw1, d_pw2, d_cfm, d_bpw2, hv, cfm, ghist, brow, browf, ones1, onesm):
        nc, s = self.nc, self.sch
        NS = 256
        w2 = self.arena[:, 0:2, :].rearrange("p s (k n) -> p (s k) n", n=D)
        hn = self.arena[:, 2, :].rearrange("p (k n) -> p k n", n=512)[:, :, 0:NS]
        w1buf = self.arena[:, 3:5, :].rearrange("p s (b k n) -> p (s b) k n", b=2, k=KC)
        scr = self.arena[:, 5, :].bitcast(F32)
        hB = scr[:, 0:NS]
        sq = [scr[:, NS:2 * NS], scr[:, 2 * NS:3 * NS]]
        tps = [scr[:, (3 + i) * NS:(4 + i) * NS] for i in range(4)]
        NT_ACT = 12
        self.tapi = 0
        s.dma("pool", w2, d_pw2[layer].rearrange("(k p) n -> p k n", p=128), writes=[("arena", 0), ("arena", 1)])
        s.dma("sp", cfm[:], d_cfm[layer], writes=["cfm"])
        s.dma("sp", browf[:], d_bpw2[layer:layer + 1, :], writes=["browf"])
        self.load_lnb(layer, 0)
        b1 = cfm[:, 0:16]
        wdw = cfm[:, 16:16 + 248].rearrange("p (c j) -> p c j", j=CONVW)
        bdw = cfm[:, 264:272]
        lng = cfm[:, 272:280]
        lnbt = cfm[:, 280:288]

        gbuf = self.sb(f"gbuf{layer}", [128, 2, 30 + NS], F32, es)
        h = self.sb(f"h{layer}", [128, KC, NS], F32, es)
        sgt = self.sb(f"sgt{layer}", [128, 2, NS], F32, es)
        mean = self.sb(f"mean{layer}", [128, NS], F32, es)
        rstd = self.sb(f"rstd{layer}", [128, NS], F32, es)

        subs = []
        for i in range(2):
            subs.append(("halo", [16 + 2 * i, 17 + 2 * i]))
        for p in range(NPIECE):
            for i in range(2):
                subs.append((("main", p, i), [4 * p + 2 * i, 4 * p + 2 * i + 1]))
        self.w1i = 0
        for kind, tiles in subs:
            full = (kind != "halo") or (layer == 0)
            tok = slice(tiles[0] * 128, tiles[0] * 128 + NS)
            xk = [("xT", t) for t in tiles]
            def mm(c, kind=kind, tiles=tiles, tok=tok, xk=xk):
                wb = self.w1i % 4
                self.w1i += 1
                wt = w1buf[:, wb]
                s.dma("sp", wt.rearrange("p k n -> p (k n)"), self.w1s[layer, c], reads=[("w1s", layer, c)], writes=[("w1", wb)])
                pb = self.pp[c % 2]
                pa, pg = pb[:, 0:NS], pb[:, 512:512 + NS]
                ka, kg = ("pp", c % 2, 0), ("pp", c % 2, 1)
                for k in range(KC):
                    s.op("pe", lambda k=k: nc.tensor.matmul(pa, wt[:, k, 0:128], self.xT[:, k, tok], start=(k == 0), stop=(k == KC - 1)),
                         reads=xk + [("w1", wb)], writes=[ka])
                for k in range(KC):
                    s.op("pe", lambda k=k: nc.tensor.matmul(pg, wt[:, k, 128:256], self.xT[:, k, tok], start=(k == 0), stop=(k == KC - 1)),
                         reads=xk + [("w1", wb)], writes=[kg])

            def glu(c, kind=kind, tiles=tiles):
                pb = self.pp[c % 2]
                pa, pg = pb[:, 0:NS], pb[:, 512:512 + NS]
                ka, kg = ("pp", c % 2, 0), ("pp", c % 2, 1)
                gb = c % 2
                gk = ("gbuf", gb)
                s.op("act", lambda: nc.scalar.activation(out=sgt[:, gb, :], in_=pg, func=AF.Sigmoid, bias=b1[:, 8 + c:9 + c]),
                     reads=[kg, "cfm"], writes=[("sgt", gb)])
                if kind == "halo":
                    s.op("dve", lambda: nc.vector.memset(gbuf[:, gb, 0:30], 0.0), writes=[gk])
                else:
                    p = kind[1]
                    s.op("act", lambda: nc.scalar.copy(gbuf[:, gb, 0:30], ghist[:, p, c, :]),
                         reads=[("ghist", p)], writes=[gk])
                s.op("dve", lambda: nc.vector.scalar_tensor_tensor(gbuf[:, gb, 30:30 + NS], pa, b1[:, c:c + 1], sgt[:, gb, :], ALU.add, ALU.mult),
                     reads=[ka, ("sgt", gb), "cfm"], writes=[gk])
                if kind == "halo":
                    for i, t in enumerate(tiles):
                        p = t - 16
                        s.op("dve", lambda p=p, i=i: nc.vector.tensor_scalar(ghist[:, p, c, :], gbuf[:, gb, 30 + 128 * i + 98:30 + 128 * (i + 1)], hv[:, p:p + 1], None, ALU.mult),
                             reads=[gk, "hv"], writes=[("ghist", p)])
                elif kind[2] == 0:
                    p = kind[1]
                    s.op("act", lambda: nc.scalar.copy(ghist[:, p, c, :], gbuf[:, gb, NS:NS + 30]),
                         reads=[gk], writes=[("ghist", p)])

            def conv(c):
                gb = c % 2
                gk = ("gbuf", gb)
                hk = ("h", c)
                pacc = self.pp[3][:, (c % 2) * 512:(c % 2) * 512 + NS]
                pk3 = ("pp", 3, c % 2)
                act_taps = list(range(CONVW - NT_ACT, CONVW))
                for i, j in enumerate(act_taps):
                    tb = self.tapi % 4
                    self.tapi += 1
                    tbuf, tkey = tps[tb], ("tap", tb)
                    s.op("act", lambda j=j, tbuf=tbuf: nc.scalar.activation(out=tbuf, in_=gbuf[:, gb, j:j + NS], func=AF.Copy, scale=wdw[:, c, j:j + 1]),
                         reads=[gk, "cfm"], writes=[tkey])
                    s.op("pe", lambda i=i, tbuf=tbuf: nc.tensor.matmul(pacc, self.ident[:], tbuf, start=(i == 0), stop=(i == len(act_taps) - 1)),
                         reads=[tkey, "ident"], writes=[pk3])
                s.op("dve", lambda: nc.vector.tensor_scalar(h[:, c, :], gbuf[:, gb, 0:NS], wdw[:, c, 0:1], bdw[:, c:c + 1], ALU.mult, ALU.add),
                     reads=[gk, "cfm"], writes=[hk])
                s.op("dve", lambda: nc.vector.tensor_scalar(hB, gbuf[:, gb, 1:1 + NS], wdw[:, c, 1:2], None, ALU.mult),
                     reads=[gk, "cfm"], writes=["hB"])
                for j in range(2, CONVW - NT_ACT):
                    acc, ak = (h[:, c, :], hk) if j % 2 == 0 else (hB, "hB")
                    s.op("dve", lambda j=j, acc=acc: nc.vector.scalar_tensor_tensor(acc, gbuf[:, gb, j:j + NS], wdw[:, c, j:j + 1], acc, ALU.mult, ALU.add),
                         reads=[gk, "cfm", ak], writes=[ak])
                s.op("dve", lambda: nc.vector.tensor_tensor(h[:, c, :], h[:, c, :], hB, ALU.add), reads=[hk, "hB"], writes=[hk])
                s.op("dve", lambda: nc.vector.tensor_tensor(h[:, c, :], h[:, c, :], pacc, ALU.add), reads=[hk, pk3], writes=[hk])

            def stats(c):
                s.op("act", lambda: nc.scalar.activation(out=sq[c % 2], in_=h[:, c, :], func=AF.Square),
                     reads=[("h", c)], writes=[("sq", c % 2)])
                s.op("pe", lambda: nc.tensor.matmul(self.pp[2][:, 0:NS], onesm[:], h[:, c, :], start=(c == 0), stop=(c == KC - 1)),
                     reads=[("h", c), "onesm"], writes=[("pp", 2, 0)])
                s.op("pe", lambda: nc.tensor.matmul(self.pp[2][:, 512:512 + NS], onesm[:], sq[c % 2], start=(c == 0), stop=(c == KC - 1)),
                     reads=[("sq", c % 2), "onesm"], writes=[("pp", 2, 1)])

            mm(0)
            mm(1)
            glu(0)
            for c in range(KC):
                if c + 2 < KC:
                    mm(c + 2)
                if c + 1 < KC:
                    glu(c + 1)
                if full:
                    if c > 0:
                        stats(c - 1)
                    conv(c)
            if full:
                stats(KC - 1)
            if not full or CUT <= 3:
                continue
            s.op("act", lambda: nc.scalar.copy(mean[:], self.pp[2][:, 0:NS]), reads=[("pp", 2, 0)], writes=["mean"])
            s.op("act", lambda: nc.scalar.activation(out=rstd[:], in_=self.pp[2][:, 0:NS], func=AF.Square), reads=[("pp", 2, 0)], writes=["rstd"])
            s.op("dve", lambda: nc.vector.scalar_tensor_tensor(rstd[:], rstd[:], -1.0, self.pp[2][:, 512:512 + NS], ALU.mult, ALU.add),
                 reads=["rstd", ("pp", 2, 1)], writes=["rstd"])
            s.op("dve", lambda: nc.vector.tensor_scalar(sgt[:, 1, :], rstd[:], LN_EPS, None, ALU.add), reads=["rstd"], writes=[("sgt", 1)])
            self.rsqrt_dve(rstd[:], sgt[:, 1, :], sgt[:, 0, :], [("sgt", 1)], "rstd", ("sgt", 0))
            for c in range(KC):
                hk = ("h", c)
                s.op("dve", lambda c=c: nc.vector.tensor_tensor(h[:, c, :], h[:, c, :], mean[:], ALU.subtract), reads=[hk, "mean"], writes=[hk])
                s.op("dve", lambda c=c: nc.vector.tensor_tensor(h[:, c, :], h[:, c, :], rstd[:], ALU.mult), reads=[hk, "rstd"], writes=[hk])
                s.op("act", lambda c=c: nc.scalar.activation(out=hn[:, c, :], in_=h[:, c, :], func=AF.Silu, bias=lnbt[:, c:c + 1], scale=lng[:, c:c + 1]),
                     reads=[hk, "cfm"], writes=["hn", ("arena", 2)])
            if CUT <= 4:
                continue
            for i, t in enumerate(tiles):
                py = self.pp[3] if False else self.pp[2 + (i % 2)]
                pidx = 2 + (i % 2)
                yk = [("pp", pidx, 0), ("pp", pidx, 1)]
                for hh in range(2):
                    for c in range(KC):
                        s.op("pe", lambda c=c, hh=hh, py=py, i=i: nc.tensor.matmul(py[:, hh * 512:(hh + 1) * 512], hn[:, c, i * 128:(i + 1) * 128], w2[:, c, hh * 512:(hh + 1) * 512], start=(c == 0), stop=False),
                             reads=["hn", ("arena", 0), ("arena", 1)], writes=[yk[hh]])
                    s.op("pe", lambda hh=hh, py=py: nc.tensor.matmul(py[:, hh * 512:(hh + 1) * 512], ones1[:], browf[:, hh * 512:(hh + 1) * 512], start=False, stop=True),
                         reads=["ones1", "browf"], writes=[yk[hh]])
                s.op("dve", lambda t=t, py=py: nc.vector.scalar_tensor_tensor(self.x[:, t, :], self.x[:, t, :], ALPHA, py[:], ALU.mult, ALU.add),
                     reads=yk + [("x", t)], writes=[("x", t)])
                if CUT <= 5:
                    continue
                self.ln_tile(t, None, 1, do_xT=(CUT > 6), do_router=(CUT > 7))

    def load_wq(self, j):
        s = self.sch
        wq = self.arena[:, 4:6, :].rearrange("p s (k n) -> p (s k) n", n=D)
        for st in range(2):
            for h in range(NH):
                s.dma("pool", wq[:, :, h * 128 + st * 64:h * 128 + st * 64 + 64],
                      self.d_wq[j, :, st * 512 + h * 64:st * 512 + h * 64 + 64].rearrange("(k p) n -> p k n", p=128),
                      writes=[("arena", 4), ("arena", 5)])

    def kv_proj(self, es, d_wkv, o_kT, o_v):
        nc, s = self.nc, self.sch
        wk = self.arena[:, 0:2, :].rearrange("p s (k n) -> p (s k) n", n=D)
        wv = self.arena[:, 2:4, :].rearrange("p s (k n) -> p (s k) n", n=D)
        s.dma("pool", wv, d_wkv[:, D:2 * D].rearrange("(k p) n -> p k n", p=128), writes=[("arena", 2), ("arena", 3)])
        for st in range(2):
            for h in range(NH):
                s.dma("pool", wk[:, :, h * 128 + st * 64:h * 128 + st * 64 + 64],
                      d_wkv[:, st * 512 + h * 64:st * 512 + h * 64 + 64].rearrange("(k p) n -> p k n", p=128),
                      writes=[("arena", 0), ("arena", 1)])
        self.load_wq(0)
        vdst = self.kvsrc[:, 512:1024].rearrange("(h c p) (b v) -> c b p h v", h=NH, c=NPIECE, p=128, b=4)
        kst = self.sb("kst", [128, 2, 512], BF16, es)
        vst = self.sb("vst", [128, 2, D], BF16, es)
        vkeys = []
        for t in range(NT_MAIN):
            pb = self.pp[2 + t % 2]
            yk = [("pp", 2 + t % 2, 0), ("pp", 2 + t % 2, 1)]
            for hh in range(2):
                for k in range(KC):
                    s.op("pe", lambda k=k, hh=hh, pb=pb, t=t: nc.tensor.matmul(pb[:, hh * 512:(hh + 1) * 512], self.xT[:, k, t * 128:(t + 1) * 128], wv[:, k, hh * 512:(hh + 1) * 512], start=(k == 0), stop=(k == KC - 1)),
                         reads=[("xT", t), ("arena", 2), ("arena", 3)], writes=[yk[hh]])
            s.op("act", lambda pb=pb, t=t: nc.scalar.copy(vst[:, t % 2, :], pb[:]), reads=yk, writes=[("vst", t % 2)])
            s.dma("sp", vdst[t // 4, t % 4], vst[:, t % 2, :].rearrange("p (h v) -> p h v", v=VD), reads=[("vst", t % 2)], writes=[("kvsrc", "v", t)])
            vkeys.append(("kvsrc", "v", t))
        it = 0
        for h in range(NH):
            kkeys = []
            for ch in range(4):
                tok = slice(ch * 512, ch * 512 + 512)
                pb = self.pp[it % 2]
                pk = ("pp", it % 2, 0)
                for k in range(KC):
                    s.op("pe", lambda k=k, pb=pb, h=h: nc.tensor.matmul(pb[:, 0:512], wk[:, k, h * 128:(h + 1) * 128], self.xT[:, k, tok], start=(k == 0), stop=(k == KC - 1)),
                         reads=[("xT", t) for t in range(ch * 4, ch * 4 + 4)] + [("arena", 0), ("arena", 1)], writes=[pk])
                s.op("act", lambda pb=pb, it=it: nc.scalar.copy(kst[:, it % 2, :], pb[:, 0:512]), reads=[pk], writes=[("kst", it % 2)])
                r0 = (h * 4 + ch) * 128
                s.dma("sp", self.kvsrc[r0:r0 + 128, 0:512], kst[:, it % 2, :], reads=[("kst", it % 2)], writes=[("kvsrc", "k", it)])
                kkeys.append(("kvsrc", "k", it))
                it += 1
            self.kv_exchange_slice(h, vkeys + kkeys)

    def kv_exchange_slice(self, i, keys):
        nc, s = self.nc, self.sch
        s._deps("pool", keys, [])
        RS = KVROWS // KVSPLIT
        nc.gpsimd.collective_compute("AllGather", ALU.bypass, replica_groups=[[0, 1, 2, 3], [4, 5, 6, 7]],
                                     ins=[self.kvsrc[i * RS:(i + 1) * RS, :]],
                                     outs=[self.kvall[i * 4 * RS:(i + 1) * 4 * RS, :]]).then_inc(self.cc_sem)

    USLOT = [4, 8, 12, 16]
    NUNIT = 40

    def attention_layer(self, layer, j):
        nc, s = self.nc, self.sch
        lam_init = 0.8 - 0.6 * math.exp(-0.3 * layer)
        wq = self.arena[:, 4:6, :].rearrange("p s (k n) -> p (s k) n", n=D)
        wo = self.arena[:, 4:6, :].rearrange("p s (k n) -> p (s k) n", n=D)
        if j > 0:
            self.load_wq(j)
        a2 = self.arena[:, 2, :]
        a3 = self.arena[:, 3, :]
        Ebuf = a2[:, 0:3072].rearrange("p (b s n) -> p b s n", b=3, s=2)
        a3f = a3.bitcast(F32)
        gt = a3f[:, 0:1024]
        tmpf = a3f[:, 1024:2048]
        a0 = self.arena[:, 0, :]
        a1f = self.arena[:, 1, :].bitcast(F32)
        KVb = a0.rearrange("p (b n) -> p b n", n=1024)
        self.KVb = KVb
        KTq = KVb[:, :, 0:512]
        Vb = KVb[:, :, 512:1024]
        of = a1f[:, 0:512]
        t2 = a1f[:, 512:1024]
        rs = a1f[:, 1024:1536]
        rz = self.xTf[0:1, :, :].rearrange("p c n -> p (c n)")
        self._attention_body(layer, j, lam_init, wq, wo, Ebuf, gt, tmpf, Vb, KTq, of, t2, rs, rz)

    def _attention_body(self, layer, j, lam_init, wq, wo, Ebuf, gt, tmpf, Vb, KTq, of, t2, rs, rz):
        nc, s = self.nc, self.sch
        l4 = self.lamt[:].rearrange("p (a b d) -> p a b d", a=2, b=2)
        lw = self.lamw
        s.op("dve", lambda: nc.vector.tensor_tensor(self.lamt[:, 0:128].rearrange("p (a d) -> p a d", a=2), l4[:, :, 0, :], l4[:, :, 1, :], ALU.mult),
             reads=["lamt"], writes=["lamt"])
        s.op("dve", lambda: nc.vector.tensor_reduce(lw[:, 0:2], self.lamt[:, 0:128].rearrange("p (a d) -> p a d", a=2), AX.X, ALU.add),
             reads=["lamt"], writes=["lamw"])
        s.op("act", lambda: nc.scalar.activation(out=lw[:, 2:4], in_=lw[:, 0:2], func=AF.Exp), reads=["lamw"], writes=["lamw"])
        s.op("dve", lambda: nc.vector.tensor_tensor(lw[:, 4:5], lw[:, 3:4], lw[:, 2:3], ALU.subtract), reads=["lamw"], writes=["lamw"])
        s.op("dve", lambda: nc.vector.tensor_scalar(lw[:, 5:6], lw[:, 4:5], -lam_init, None, ALU.add), reads=["lamw"], writes=["lamw2"])
        s.op("pe", lambda: nc.tensor.matmul(self.pp[0][:, 0:1], self.ones1f[:], lw[:, 5:6], start=True, stop=True),
             reads=["ones1f", "lamw2"], writes=[("pp", 0, 0)])
        s.op("act", lambda: nc.scalar.copy(self.lamb[:, 0:1], self.pp[0][:, 0:1]), reads=[("pp", 0, 0)], writes=["lamb"])
        s.op("dve", lambda: nc.vector.tensor_scalar(self.sgl[:, 2:3], self.sgl[:, j:j + 1], 1.0 - lam_init, None, ALU.mult), reads=["sgl"], writes=["sgl2"])
        it = 0
        for h in range(NH):
            for sl in range(NPIECE):
                tok = slice(sl * 512, sl * 512 + 512)
                pb = self.pp[it % 2]
                pk = ("pp", it % 2, 0)
                for k in range(KC):
                    s.op("pe", lambda k=k, pb=pb, h=h, tok=tok: nc.tensor.matmul(pb[:, 0:512], wq[:, k, h * 128:(h + 1) * 128], self.xT[:, k, tok], start=(k == 0), stop=(k == KC - 1)),
                         reads=[("xT", t) for t in range(sl * 4, sl * 4 + 4)] + [("arena", 4), ("arena", 5)], writes=[pk])
                s.op("act", lambda pb=pb, h=h, sl=sl: nc.scalar.activation(out=self.qap(h, sl), in_=pb[:, 0:512], func=AF.Copy, scale=HD ** -0.5),
                     reads=[pk], writes=[("qT", h, sl)])
                it += 1
        s.dma("pool", wo, self.d_wo[j].rearrange("(k p) n -> p k n", p=128), writes=[("arena", 4), ("arena", 5)])
        if j == 0:
            self.kv_exchange()
        s.barrier()
        NU = self.NUNIT
        blocks = []
        units = []
        for h in range(NH):
            ubase = 0
            for sl in range(NPIECE):
                for d in reversed(range(self.USLOT[sl])):
                    ui = len(units)
                    units.append((h, ubase + d))
                    for kb in (range(4) if d > 0 else reversed(range(4))):
                        blocks.append(dict(h=h, sl=sl, d=d, kb=kb, unit=ubase + d, ui=ui,
                                           q0=(kb * 128 if d == 0 else 0),
                                           near=(d == 0) or (d == 1 and kb == 3),
                                           first=(d == self.USLOT[sl] - 1 and kb == 0),
                                           last=(d == 0 and kb == 0)))
                ubase += self.USLOT[sl]
        pO, pZ = self.pp[2], self.pp[3]
        kO = [("pp", 2, 0), ("pp", 2, 1)]
        kZ = [("pp", 3, 0), ("pp", 3, 1)]
        st8 = {"sit": 0, "eit": 0, "uload": 0, "nit": 0}
        zS = self.xTf[:].rearrange("p c n -> p (c n)")
        oS = self.lnb[:, 0, :]
        tmpfs = [(tmpf, "tmpf"), (self.lnb[:, 1, :], "tmpf2")]
        pending = []

        def load_units(upto):
            while st8["uload"] <= min(upto, len(units) - 1):
                ui = st8["uload"]
                hh, un = units[ui]
                ub_ = ui % 4
                s.idma(self.KVb[:, ub_, :], self.kvall[:, :], self.idxb[:, hh, un:un + 1], reads=["idx"], writes=[("KTq", ub_), ("Vb", ub_)])
                st8["uload"] += 1

        def emit_S(bk):
            if bk["kb"] == 0:
                load_units(bk["ui"] + 2)
            h, sl, kb, q0 = bk["h"], bk["sl"], bk["kb"], bk["q0"]
            ub_ = bk["ui"] % 4
            nq = 512 - q0
            si = st8["sit"] % 2
            st8["sit"] += 1
            pS = self.pp[si]
            kS = [("pp", si, 0), ("pp", si, 1)]
            bk["pS"], bk["kS"] = pS, kS
            for st in range(2):
                s.op("pe", lambda st=st: nc.tensor.matmul(
                    pS[:, st * 512:st * 512 + nq], KTq[st * 64:(st + 1) * 64, ub_, kb * 128:(kb + 1) * 128],
                    self.qap(h, sl)[st * 64:(st + 1) * 64, q0:512], start=True, stop=True),
                    reads=[("KTq", ub_), ("qT", h, sl)], writes=[kS[st]])

        def emit_head_setup(h):
            s.dma("sp", gt, self.d_gt[h], writes=["gt"])
            s.op("dve", lambda: nc.vector.tensor_scalar(self.fb[:], self.ub[:], self.b31[:, h:h + 1], None, ALU.add),
                 reads=["ub", "b31"], writes=["fb"])

        def emit_exp_pv(bk):
            h, sl, d, kb, q0, unit = bk["h"], bk["sl"], bk["d"], bk["kb"], bk["q0"], bk["unit"]
            ub_ = bk["ui"] % 4
            nq = 512 - q0
            pS, kS = bk["pS"], bk["kS"]
            eb = st8["eit"] % 3
            st8["eit"] += 1
            pS3 = pS[:].rearrange("p (s n) -> p s n", s=2)[:, :, 0:nq]
            E3 = Ebuf[:, eb, :, 0:nq]
            if bk["near"]:
                m0 = q0 + d * 512 - kb * 128 + 384
                tbuf, tkey = tmpfs[st8["nit"] % 2]
                st8["nit"] += 1
                tm3 = tbuf.rearrange("p (s n) -> p s n", s=2)[:, :, 0:nq]
                g3 = gt[:, m0:m0 + nq].unsqueeze(1).to_broadcast([128, 2, nq])
                s.op("dve", lambda: nc.vector.scalar_tensor_tensor(tm3, pS3, self.ub[:, unit:unit + 1], g3, ALU.add, ALU.add),
                     reads=kS + ["gt", "ub"], writes=[tkey])
                s.op("act", lambda: nc.scalar.activation(out=E3, in_=tm3, func=AF.Exp),
                     reads=[tkey], writes=[("E", eb)])
            else:
                s.op("act", lambda: nc.scalar.activation(out=E3, in_=pS3, func=AF.Exp, bias=self.fb[:, unit:unit + 1]),
                     reads=kS + ["fb"], writes=[("E", eb)])
            first, last = bk["first"], bk["last"]
            for st in range(2):
                s.op("pe", lambda st=st: nc.tensor.matmul(
                    pO[:, st * 512 + q0:st * 512 + 512], Vb[:, ub_, kb * 128:(kb + 1) * 128], Ebuf[:, eb, st, 0:nq], start=first, stop=last),
                    reads=[("Vb", ub_), ("E", eb)], writes=[kO[st]])
                s.op("pe", lambda st=st: nc.tensor.matmul(
                    pZ[:, st * 512 + q0:st * 512 + 512], self.onesb[:], Ebuf[:, eb, st, 0:nq], start=first, stop=last),
                    reads=["onesb", ("E", eb)], writes=[kZ[st]])

        def emit_norm(h, sl):
            qcols = sl * 512
            s.op("act", lambda: nc.scalar.copy(zS, pZ[:]), reads=kZ, writes=["xTf"])
            s.op("act", lambda: nc.scalar.copy(oS, pO[:]), reads=kO, writes=["oS"])

            def dveA():
                s.op("dve", lambda: nc.vector.reciprocal(zS, zS), reads=["xTf"], writes=["xTf"])
                s.op("dve", lambda: nc.vector.tensor_tensor(of[:], oS[:, 0:512], zS[:, 0:512], ALU.mult), reads=["oS", "xTf"], writes=["of"])
                s.op("dve", lambda: nc.vector.scalar_tensor_tensor(t2[:], oS[:, 512:1024], self.lamb[:, 0:1], zS[:, 512:1024], ALU.mult, ALU.mult),
                     reads=["oS", "xTf", "lamb"], writes=["t2"])
                s.op("dve", lambda: nc.vector.tensor_tensor(of[:], of[:], t2[:], ALU.add), reads=["of", "t2"], writes=["of"])

            def actSq():
                s.op("act", lambda: nc.scalar.activation(out=t2[:], in_=of[:], func=AF.Square), reads=["of"], writes=["t2"])

            def dveB():
                qi = st8["sit"] % 2
                pQ = self.pp[qi]
                kQ = ("pp", qi, 0)
                s.op("pe", lambda: nc.tensor.matmul(pQ[:, 0:512], self.onesv[:], t2[:], start=True, stop=True), reads=["onesv", "t2"], writes=[kQ])
                s.op("dve", lambda: nc.vector.tensor_scalar(zS[:, 0:512], pQ[:, 0:512], LN_EPS, None, ALU.add), reads=[kQ], writes=["xTf"])
                self.rsqrt_dve(rs[:], zS[:, 0:512], t2[:], ["xTf"], "rs", "t2")
                s.op("dve", lambda: nc.vector.tensor_tensor(of[:], of[:], rs[:], ALU.mult), reads=["of", "rs"], writes=["of"])

            def actOut():
                s.op("act", lambda: nc.scalar.activation(out=self.xT[:, h, qcols:qcols + 512], in_=of[:], func=AF.Copy, scale=self.sgl[:, 2:3]),
                     reads=["of", "sgl2"], writes=[("xT", t) for t in range(sl * 4, sl * 4 + 4)])

            pending.append([1, dveA])
            pending.append([7, actSq])
            pending.append([8, dveB])
            pending.append([15, actOut])

        def run_pending(flush=False):
            for p in pending:
                p[0] -= 1
            pending.sort(key=lambda p: p[0])
            while pending and (flush or pending[0][0] <= 0):
                pending.pop(0)[1]()

        emit_S(blocks[0])
        for i, bk in enumerate(blocks):
            if i + 1 < len(blocks):
                emit_S(blocks[i + 1])
            if bk["sl"] == 0 and bk["first"]:
                emit_head_setup(bk["h"])
            emit_exp_pv(bk)
            run_pending()
            if bk["last"]:
                emit_norm(bk["h"], bk["sl"])
        run_pending(flush=True)
        self.load_lnb(layer, 0)

        def oproj(t):
            py = self.pp[t % 2]
            yk = [("pp", t % 2, 0), ("pp", t % 2, 1)]
            for hh in range(2):
                for h in range(NH):
                    s.op("pe", lambda h=h, hh=hh: nc.tensor.matmul(py[:, hh * 512:(hh + 1) * 512], self.xT[:, h, t * 128:(t + 1) * 128], wo[:, h, hh * 512:(hh + 1) * 512], start=(h == 0), stop=(h == NH - 1)),
                         reads=[("xT", t), ("arena", 4), ("arena", 5)], writes=[yk[hh]])

        oproj(0)
        for t in range(NT_MAIN):
            if t + 1 < NT_MAIN:
                oproj(t + 1)
            py = self.pp[t % 2]
            yk = [("pp", t % 2, 0), ("pp", t % 2, 1)]
            s.op("dve", lambda: nc.vector.scalar_tensor_tensor(self.x[:, t, :], self.x[:, t, :], ALPHA, py[:], ALU.mult, ALU.add),
                 reads=yk + [("x", t)], writes=[("x", t)])
            self.ln_tile(t, None, 1, do_xT=True, do_router=True)


def _core_tokens(c):
    b, r = divmod(c, 4)
    pos = piece_positions(r)
    return b, pos


def t5_bucket_np(n):
    n = np.maximum(n, 0)
    nf = np.maximum(n, 1).astype(np.float32)
    large = 16 + (np.log(nf / np.float32(16)) / np.float32(math.log(128 / 16)) * np.float32(16)).astype(np.int32)
    large = np.minimum(large, 31)
    return np.where(n < 16, n, large)


def prep_F(inp):
    x = np.asarray(inp["x"], dtype=np.float32)
    rel_bias = np.asarray(inp["rel_bias"], np.float32)
    ki = np.arange(128)[:, None]
    m = np.arange(1024)[None, :]
    rel = m - ki - 384
    bidx = t5_bucket_np(rel)
    gtab = np.empty((NH, 128, 1024), np.float32)
    for h in range(NH):
        g = rel_bias[bidx, h]
        gtab[h] = np.where(rel >= 0, g, np.float32(NEG))
    shared = {
        "ident": np.eye(128, dtype=np.float32),
        "rw": np.ascontiguousarray(np.asarray(inp["router_w"], np.float32).reshape(KC, 128, NE).transpose(1, 0, 2).reshape(128, KC * NE)),
        "rbias": np.ascontiguousarray(np.broadcast_to(np.asarray(inp["router_bias"], np.float32)[None, :], (128, NE))),
        "moe_w_gate": np.asarray(inp["moe_w_gate"], np.float32),
        "moe_w_up": np.asarray(inp["moe_w_up"], np.float32),
        "moe_w_down": np.asarray(inp["moe_w_down"], np.float32),
        "a_w_pw1": np.asarray(inp["a_w_pw1"], np.float32),
        "a_w_pw2": np.asarray(inp["a_w_pw2"], np.float32),
        "a_b_pw2": np.asarray(inp["a_b_pw2"], np.float32),
        "w_kv": np.asarray(inp["w_kv"], np.float32),
        "gtab": gtab,
        "b31r": np.ascontiguousarray(np.broadcast_to(rel_bias[31][None, :], (128, NH))),
        "b_w_q": np.asarray(inp["b_w_q"], np.float32),
        "b_w_o": np.asarray(inp["b_w_o"], np.float32),
        "b_lambda": np.asarray(inp["b_lambda"], np.float32).reshape(2, 1, 256),
        "sublng": np.ascontiguousarray(np.asarray(inp["b_subln_g"], np.float32).T),
    }
    lnb = np.empty((DEPTH, 4, 128, D), np.float32)
    for l in range(DEPTH):
        for i, k in enumerate(("ln_mix_g", "ln_mix_b", "ln_ffn_g", "ln_ffn_b")):
            lnb[l, i] = np.asarray(inp[k], np.float32)[l][None, :]
    shared["lnb"] = lnb
    cfm = np.empty((N_A, 128, 288), np.float32)
    for l in range(N_A):
        cfm[l, :, 0:16] = np.asarray(inp["a_b_pw1"], np.float32)[l].reshape(16, 128).T
        wdw = np.asarray(inp["a_w_dw"], np.float32)[l]
        cfm[l, :, 16:264] = wdw.reshape(CONVW, KC, 128).transpose(2, 1, 0).reshape(128, KC * CONVW)
        cfm[l, :, 264:272] = np.asarray(inp["a_b_dw"], np.float32)[l].reshape(KC, 128).T
        cfm[l, :, 272:280] = np.asarray(inp["a_ln_g"], np.float32)[l].reshape(KC, 128).T
        cfm[l, :, 280:288] = np.asarray(inp["a_ln_b"], np.float32)[l].reshape(KC, 128).T
    shared["cfm"] = cfm
    owner = {}
    for r in range(4):
        for p, g in enumerate(piece_positions(r)):
            owner[g] = (r, p)
    NU = Prog.NUNIT
    parr = np.arange(128, dtype=np.int64)
    maps = []
    for c in range(NCORES):
        b, pos = _core_tokens(c)
        xin = np.zeros(((NT_MAIN + NT_HALO) * 128, D), np.float32)
        hvv = np.zeros((128, NPIECE), np.float32)
        for p, ps_ in enumerate(pos):
            xin[p * 512:(p + 1) * 512] = x[b, ps_ * 512:(ps_ + 1) * 512]
            if ps_ > 0:
                xin[(16 + p) * 128:(17 + p) * 128] = x[b, ps_ * 512 - 128:ps_ * 512]
                hvv[:, p] = 1.0
        ub = np.zeros((128, NU), np.float32)
        kvidx = np.zeros((NH, 128, NU), np.int32)
        u = 0
        for sl in range(NPIECE):
            for d in range(Prog.USLOT[sl]):
                g = pos[sl] - d
                if g < 0:
                    ub[:, u] = NEG
                    g = 0
                r2, p2 = owner[g]
                RS = KVROWS // KVSPLIT
                for h in range(NH):
                    row = (h * 4 + p2) * 128
                    kvidx[h, :, u] = (row // RS) * 4 * RS + r2 * RS + row % RS + parr
                u += 1
        mm = dict(shared)
        mm["xin"] = xin
        mm["halo_valid"] = hvv
        mm["ubias"] = ub
        mm["kvidx"] = kvidx
        maps.append(mm)
    return maps


_PROGS = {}


def get_prog():
    if "F" not in _PROGS:
        _PROGS["F"] = Prog("F").build()
    return _PROGS["F"]


def kernel(**inputs):
    maps = prep_F(inputs)
    nc = get_prog()
    res = run_bass_kernel_spmd(nc, maps, core_ids=list(range(NCORES))).results
    out = np.empty((BATCH, SEQ, D), np.float32)
    for c in range(NCORES):
        b, pos = _core_tokens(c)
        xo = np.asarray(res[c]["x_out"], np.float32)
        for p, g in enumerate(pos):
            out[b, g * 512:(g + 1) * 512] = xo[p * 512:(p + 1) * 512]
    return out
```
